# Optimizing a Trainium2 kernel written in Bass

```python
import math
import jax
import jax.numpy as jnp
from jax import lax
import numpy as np

D_MODEL = 1024
BATCH = 32
SEQ = 2048
DEPTH = 2

N_A_LAYERS = (DEPTH + 1) // 2
N_B_LAYERS = DEPTH - N_A_LAYERS

GDN_HEADS = 8
GDN_HEAD_DIM = 128
GDN_WIDTH = GDN_HEADS * GDN_HEAD_DIM
GDN_CONV = 4
GDN_CHUNK = 64

NSA_HEADS = 16
NSA_GROUPS = 4
NSA_HPG = NSA_HEADS // NSA_GROUPS
NSA_HEAD_DIM = 64
NSA_WIDTH = NSA_HEADS * NSA_HEAD_DIM
CMP_BLOCK = 32
CMP_STRIDE = 16
CMP_HIDDEN = 2 * NSA_HEAD_DIM
SEL_BLOCK = 64
TOP_N = 16
WINDOW = 512
Q_BLOCK = 128
N_BRANCHES = 3
N_KV_PARTS = 2 * N_BRANCHES

REL_BUCKETS = 32
REL_MAX_DIST = 128

MOE_GROUPS = 4
MOE_EXPERTS_PER_GROUP = 4
MOE_EXPERTS = MOE_GROUPS * MOE_EXPERTS_PER_GROUP
MOE_TOP_K = 2
MOE_HIDDEN = 256

DEEPNORM_ALPHA = (2 * DEPTH) ** 0.25
DEEPNORM_BETA = (8 * DEPTH) ** -0.25
LN_EPS = 1e-5
NORM_EPS = 1e-6
NEG_INF = -1e30

kernel_name = 'hybrid_gdn_nsa_hmoe_deepnorm'


def layer_norm(x, g, b):
    xf = x.astype(jnp.float32)
    mu = jnp.mean(xf, -1, keepdims=True)
    var = jnp.mean(jnp.square(xf - mu), -1, keepdims=True)
    return ((xf - mu) * lax.rsqrt(var + LN_EPS) * g.astype(jnp.float32) + b.astype(jnp.float32)).astype(x.dtype)


def rms_norm(x, g):
    xf = x.astype(jnp.float32)
    return xf * lax.rsqrt(jnp.mean(xf * xf, -1, keepdims=True) + NORM_EPS) * g.astype(jnp.float32)


def l2_normalize(x):
    xf = x.astype(jnp.float32)
    return xf * lax.rsqrt(jnp.sum(xf * xf, -1, keepdims=True) + NORM_EPS)


def causal_depthwise_conv(x, w):
    k, c = w.shape
    return lax.conv_general_dilated(x, w[:, None, :].astype(x.dtype), window_strides=(1,),
                                    padding=[(k - 1, 0)], dimension_numbers=('NWC', 'WIO', 'NWC'),
                                    feature_group_count=c)


def masked_softmax(s, mask):
    return jax.nn.softmax(jnp.where(mask, s, NEG_INF), axis=-1) * mask


def gated_delta_rule_chunked(q, k, v, g, beta):
    b, t, h, dk = q.shape
    dv = v.shape[-1]
    c = GDN_CHUNK
    n = t // c

    def chunks(a):
        a = jnp.moveaxis(a.astype(jnp.float32), 1, 2)
        return a.reshape((b, h, n, c) + a.shape[3:])

    q, k, v, g, beta = chunks(q), chunks(k), chunks(v), chunks(g), chunks(beta)
    gc = jnp.cumsum(g, axis=-1)
    idx = jnp.arange(c)
    causal = idx[:, None] >= idx[None, :]
    decay = jnp.exp(jnp.where(causal, gc[..., :, None] - gc[..., None, :], -jnp.inf))
    k_beta = k * beta[..., None]
    a_strict = jnp.where(idx[:, None] > idx[None, :],
                         jnp.einsum('bhncd,bhnsd->bhncs', k_beta, k) * decay, 0.0)
    tri = a_strict + jnp.eye(c, dtype=jnp.float32)
    u = lax.linalg.triangular_solve(tri, v * beta[..., None], left_side=True, lower=True, unit_diagonal=True)
    w = lax.linalg.triangular_solve(tri, k_beta * jnp.exp(gc)[..., None], left_side=True, lower=True,
                                    unit_diagonal=True)
    qk = jnp.einsum('bhncd,bhnsd->bhncs', q, k) * decay
    g_last = gc[..., -1:]
    q_dec = q * jnp.exp(gc)[..., None]
    k_dec = k * jnp.exp(g_last - gc)[..., None]
    d_last = jnp.exp(g_last[..., 0])
    xs = (jnp.moveaxis(q_dec, 2, 0), jnp.moveaxis(qk, 2, 0), jnp.moveaxis(u, 2, 0),
          jnp.moveaxis(w, 2, 0), jnp.moveaxis(k_dec, 2, 0), jnp.moveaxis(d_last, 2, 0))

    def step(state, inp):
        q_c, qk_c, u_c, w_c, k_c, d_c = inp
        v_new = u_c - jnp.einsum('bhcd,bhde->bhce', w_c, state)
        o_c = jnp.einsum('bhcd,bhde->bhce', q_c, state) + jnp.einsum('bhcs,bhse->bhce', qk_c, v_new)
        state = state * d_c[..., None, None] + jnp.einsum('bhcd,bhce->bhde', k_c, v_new)
        return state, o_c

    _, o = lax.scan(step, jnp.zeros((b, h, dk, dv), jnp.float32), xs)
    return jnp.moveaxis(o, 0, 2).reshape(b, h, t, dv).transpose(0, 2, 1, 3)


def gdn_mixer(x, w_in, conv_w, a_log, dt_bias, norm_w, w_o):
    b, t, _ = x.shape
    hk = GDN_WIDTH
    proj = x @ w_in
    qkv = jax.nn.silu(causal_depthwise_conv(proj[..., :3 * hk], conv_w))
    shp = (b, t, GDN_HEADS, GDN_HEAD_DIM)
    q = l2_normalize(qkv[..., :hk].reshape(shp)) * (GDN_HEAD_DIM ** -0.5)
    k = l2_normalize(qkv[..., hk:2 * hk].reshape(shp))
    v = qkv[..., 2 * hk:3 * hk].reshape(shp)
    z = proj[..., 3 * hk:4 * hk].reshape(shp).astype(jnp.float32)
    a = proj[..., 4 * hk:4 * hk + GDN_HEADS].astype(jnp.float32)
    bt = proj[..., 4 * hk + GDN_HEADS:].astype(jnp.float32)
    g = -jnp.exp(a_log.astype(jnp.float32)) * jax.nn.softplus(a + dt_bias.astype(jnp.float32))
    beta = jax.nn.sigmoid(bt)
    o = gated_delta_rule_chunked(q, k, v, g, beta)
    o = rms_norm(o, norm_w) * jax.nn.silu(z)
    return o.reshape(b, t, hk).astype(x.dtype) @ w_o


def t5_bucket(dist):
    n = jnp.maximum(dist, 0)
    max_exact = REL_BUCKETS // 2
    nf = jnp.maximum(n, 1).astype(jnp.float32)
    large = max_exact + (jnp.log(nf / max_exact) / math.log(REL_MAX_DIST / max_exact)
                         * (REL_BUCKETS - max_exact)).astype(jnp.int32)
    large = jnp.minimum(large, REL_BUCKETS - 1)
    return jnp.where(n < max_exact, n, large)


def t5_bias(dist, rel_tbl):
    dist = jnp.broadcast_to(dist, (NSA_GROUPS,) + dist.shape[-2:])
    tbl = rel_tbl.astype(jnp.float32).reshape(REL_BUCKETS, NSA_GROUPS, NSA_HPG)
    bias = tbl[t5_bucket(dist), jnp.arange(NSA_GROUPS)[:, None, None]]
    return bias.transpose(0, 3, 1, 2)


def compress_blocks(x, pos, w1, w2):
    b, t, g, dk = x.shape
    nc = (t - CMP_BLOCK) // CMP_STRIDE + 1
    idx = jnp.arange(nc)[:, None] * CMP_STRIDE + jnp.arange(CMP_BLOCK)[None, :]
    blk = x[:, idx] + pos[None, None, :, None, :]
    blk = blk.transpose(0, 3, 1, 2, 4).reshape(b, g, nc, CMP_BLOCK * dk)
    return jax.nn.silu(blk @ w1) @ w2


def nsa_shared_kv(h, w_kv, cmp_k_pos, cmp_k_w1, cmp_k_w2, cmp_v_pos, cmp_v_w1, cmp_v_w2):
    b, t, _ = h.shape
    kv = (h @ w_kv).reshape(b, t, N_KV_PARTS, NSA_GROUPS, NSA_HEAD_DIM)
    kc_raw, vc_raw, ks, vs, kw, vw = jnp.moveaxis(kv, 2, 0)
    kc = compress_blocks(kc_raw, cmp_k_pos, cmp_k_w1, cmp_k_w2)
    vc = compress_blocks(vc_raw, cmp_v_pos, cmp_v_w1, cmp_v_w2)
    nb = t // SEL_BLOCK

    def to_blocks(a):
        return a.transpose(0, 2, 1, 3).reshape(b, NSA_GROUPS, nb, SEL_BLOCK, NSA_HEAD_DIM)

    def pad_front(a):
        return jnp.pad(a.transpose(0, 2, 1, 3), ((0, 0), (0, 0), (WINDOW, 0), (0, 0)))

    return (kc, vc, to_blocks(ks), to_blocks(vs), pad_front(kw), pad_front(vw))


def nsa_attention(q, gates, kc, vc, ks, vs, kw, vw, rel_tbl):
    b, t = q.shape[:2]
    nc = kc.shape[2]
    nb = ks.shape[2]
    nq = t // Q_BLOCK
    n_sel = min(TOP_N, nb)
    scale = NSA_HEAD_DIM ** -0.5
    cmp_end = jnp.arange(nc) * CMP_STRIDE + CMP_BLOCK - 1
    cmp_start = jnp.arange(nc) * CMP_STRIDE
    sel_start = jnp.arange(nb) * SEL_BLOCK
    overlap = ((cmp_start[:, None] < sel_start[None, :] + SEL_BLOCK)
               & (cmp_end[:, None] >= sel_start[None, :])).astype(jnp.float32)
    g_ix = jnp.arange(NSA_GROUPS)[:, None, None]

    def per_batch(args):
        q_b, gate_b, kc_b, vc_b, ks_b, vs_b, kw_b, vw_b = args
        kc_f, vc_f = kc_b.astype(jnp.float32), vc_b.astype(jnp.float32)
        qb = q_b.reshape(nq, Q_BLOCK, NSA_GROUPS, NSA_HPG, NSA_HEAD_DIM)
        gb = gate_b.reshape(nq, Q_BLOCK, NSA_GROUPS, NSA_HPG, N_BRANCHES)

        def per_block(blk):
            q_c, gate_c, c = blk
            tq = c * Q_BLOCK + jnp.arange(Q_BLOCK)
            qf = q_c.astype(jnp.float32)
            s = jnp.einsum('qghd,gnd->ghqn', qf, kc_f) * scale + t5_bias(tq[:, None] - cmp_end[None, :], rel_tbl)
            p_cmp = masked_softmax(s, cmp_end[None, :] <= tq[:, None])
            o_cmp = jnp.einsum('ghqn,gnd->qghd', p_cmp, vc_f)
            imp = jnp.einsum('ghqn,nj->gqj', p_cmp, overlap)
            jq = (tq // SEL_BLOCK)[:, None]
            jb = jnp.arange(nb)[None, :]
            forced = (jb == 0) | (jb == jq) | (jb == jq - 1)
            imp = jnp.where(forced, jnp.inf, jnp.where(jb <= jq, imp, -jnp.inf))
            _, sel = lax.top_k(imp, n_sel)
            k_sel = ks_b[g_ix, sel].reshape(NSA_GROUPS, Q_BLOCK, n_sel * SEL_BLOCK, NSA_HEAD_DIM)
            v_sel = vs_b[g_ix, sel].reshape(NSA_GROUPS, Q_BLOCK, n_sel * SEL_BLOCK, NSA_HEAD_DIM)
            kpos = (sel[..., None] * SEL_BLOCK + jnp.arange(SEL_BLOCK)).reshape(NSA_GROUPS, Q_BLOCK, -1)
            dist = tq[None, :, None] - kpos
            s = jnp.einsum('qghd,gqkd->ghqk', qf, k_sel.astype(jnp.float32)) * scale + t5_bias(dist, rel_tbl)
            p = masked_softmax(s, (dist >= 0)[:, None])
            o_sel = jnp.einsum('ghqk,gqkd->qghd', p, v_sel.astype(jnp.float32))
            k_win = lax.dynamic_slice_in_dim(kw_b, c * Q_BLOCK, WINDOW + Q_BLOCK, axis=1).astype(jnp.float32)
            v_win = lax.dynamic_slice_in_dim(vw_b, c * Q_BLOCK, WINDOW + Q_BLOCK, axis=1).astype(jnp.float32)
            kp = c * Q_BLOCK - WINDOW + jnp.arange(WINDOW + Q_BLOCK)
            dist = tq[:, None] - kp[None, :]
            mask = (dist >= 0) & (dist < WINDOW) & (kp[None, :] >= 0)
            s = jnp.einsum('qghd,gkd->ghqk', qf, k_win) * scale + t5_bias(dist, rel_tbl)
            p = masked_softmax(s, mask)
            o_win = jnp.einsum('ghqk,gkd->qghd', p, v_win)
            gt = jax.nn.sigmoid(gate_c.astype(jnp.float32))
            return gt[..., 0:1] * o_cmp + gt[..., 1:2] * o_sel + gt[..., 2:3] * o_win

        o = lax.map(per_block, (qb, gb, jnp.arange(nq)))
        return o.reshape(t, NSA_WIDTH)

    return lax.map(per_batch, (q, gates, kc, vc, ks, vs, kw, vw))


def nsa_mixer(h, shared, w_in, w_o, rel_tbl):
    b, t, _ = h.shape
    proj = h @ w_in
    q = proj[..., :NSA_WIDTH].reshape(b, t, NSA_GROUPS, NSA_HPG, NSA_HEAD_DIM)
    gates = proj[..., NSA_WIDTH:].reshape(b, t, NSA_GROUPS, NSA_HPG, N_BRANCHES)
    kc, vc, ks, vs, kw, vw = shared
    o = nsa_attention(q, gates, kc, vc, ks, vs, kw, vw, rel_tbl)
    return o.astype(h.dtype) @ w_o


def hier_moe(x, w_grp, b_grp, w_rt, b_rt, w_gate, w_up, w_down):
    b, t, d = x.shape
    xt = x.reshape(b * t, d)
    grp_p = jax.nn.softmax((xt @ w_grp + b_grp).astype(jnp.float32), axis=-1)
    grp_prob, grp_idx = lax.top_k(grp_p, 1)
    e_logits = (xt @ w_rt + b_rt).astype(jnp.float32).reshape(-1, MOE_GROUPS, MOE_EXPERTS_PER_GROUP)
    e_logits = jnp.take_along_axis(e_logits, grp_idx[:, :, None], axis=1)[:, 0]
    top_p, top_i = lax.top_k(jax.nn.softmax(e_logits, axis=-1), MOE_TOP_K)
    top_w = grp_prob * top_p / jnp.sum(top_p, -1, keepdims=True)
    expert_id = grp_idx * MOE_EXPERTS_PER_GROUP + top_i
    gate = jnp.sum(jax.nn.one_hot(expert_id, MOE_EXPERTS, dtype=jnp.float32) * top_w[..., None], axis=1)

    def step(acc, e_in):
        wg, wu, wd, ge = e_in
        hdn = jax.nn.silu(xt @ wg) * (xt @ wu)
        return acc + ge[:, None] * (hdn @ wd).astype(jnp.float32), None

    y, _ = lax.scan(step, jnp.zeros((b * t, d), jnp.float32), (w_gate, w_up, w_down, gate.T))
    return y.astype(x.dtype).reshape(b, t, d)


def setup_inputs(seed: int = 0) -> dict:
    key = jax.random.key(seed)
    keys = iter(jax.random.split(key, 40))

    def nrm(shape, scale):
        return jax.random.normal(next(keys), shape, jnp.float32) * scale

    na, nb_l, d = N_A_LAYERS, N_B_LAYERS, D_MODEL
    dt = jnp.exp(jax.random.uniform(next(keys), (na, GDN_HEADS), jnp.float32,
                                    minval=math.log(1e-3), maxval=math.log(0.1)))
    return {
        'x': nrm((BATCH, SEQ, d), 1.0),
        'gdn_w_in': nrm((na, d, 4 * GDN_WIDTH + 2 * GDN_HEADS), d ** -0.5),
        'gdn_conv_w': nrm((na, GDN_CONV, 3 * GDN_WIDTH), GDN_CONV ** -0.5),
        'gdn_a_log': jnp.log(jax.random.uniform(next(keys), (na, GDN_HEADS), jnp.float32, minval=1.0, maxval=16.0)),
        'gdn_dt_bias': dt + jnp.log(-jnp.expm1(-dt)),
        'gdn_norm_w': 1.0 + nrm((na, GDN_HEAD_DIM), 0.01),
        'gdn_w_o': nrm((na, GDN_WIDTH, d), GDN_WIDTH ** -0.5 * DEEPNORM_BETA),
        'nsa_w_kv': nrm((d, N_KV_PARTS * NSA_GROUPS * NSA_HEAD_DIM), d ** -0.5),
        'cmp_k_pos': nrm((CMP_BLOCK, NSA_HEAD_DIM), 0.1),
        'cmp_k_w1': nrm((CMP_BLOCK * NSA_HEAD_DIM, CMP_HIDDEN), (CMP_BLOCK * NSA_HEAD_DIM) ** -0.5),
        'cmp_k_w2': nrm((CMP_HIDDEN, NSA_HEAD_DIM), CMP_HIDDEN ** -0.5),
        'cmp_v_pos': nrm((CMP_BLOCK, NSA_HEAD_DIM), 0.1),
        'cmp_v_w1': nrm((CMP_BLOCK * NSA_HEAD_DIM, CMP_HIDDEN), (CMP_BLOCK * NSA_HEAD_DIM) ** -0.5),
        'cmp_v_w2': nrm((CMP_HIDDEN, NSA_HEAD_DIM), CMP_HIDDEN ** -0.5),
        'nsa_w_in': nrm((nb_l, d, NSA_WIDTH + N_BRANCHES * NSA_HEADS), d ** -0.5),
        'nsa_w_o': nrm((nb_l, NSA_WIDTH, d), NSA_WIDTH ** -0.5 * DEEPNORM_BETA),
        'rel_bias': nrm((REL_BUCKETS, NSA_HEADS), 0.5),
        'ln_mix_g': 1.0 + nrm((DEPTH, d), 0.01),
        'ln_mix_b': nrm((DEPTH, d), 0.01),
        'ln_ffn_g': 1.0 + nrm((DEPTH, d), 0.01),
        'ln_ffn_b': nrm((DEPTH, d), 0.01),
        'moe_w_grp': nrm((DEPTH, d, MOE_GROUPS), d ** -0.5),
        'moe_b_grp': nrm((DEPTH, MOE_GROUPS), 0.01),
        'moe_w_rt': nrm((DEPTH, d, MOE_EXPERTS), d ** -0.5),
        'moe_b_rt': nrm((DEPTH, MOE_EXPERTS), 0.01),
        'moe_w_gate': nrm((DEPTH, MOE_EXPERTS, d, MOE_HIDDEN), d ** -0.5),
        'moe_w_up': nrm((DEPTH, MOE_EXPERTS, d, MOE_HIDDEN), d ** -0.5),
        'moe_w_down': nrm((DEPTH, MOE_EXPERTS, MOE_HIDDEN, d), MOE_HIDDEN ** -0.5 * DEEPNORM_BETA),
    }


def reference(x, gdn_w_in, gdn_conv_w, gdn_a_log, gdn_dt_bias, gdn_norm_w, gdn_w_o,
              nsa_w_kv, cmp_k_pos, cmp_k_w1, cmp_k_w2, cmp_v_pos, cmp_v_w1, cmp_v_w2,
              nsa_w_in, nsa_w_o, rel_bias,
              ln_mix_g, ln_mix_b, ln_ffn_g, ln_ffn_b,
              moe_w_grp, moe_b_grp, moe_w_rt, moe_b_rt, moe_w_gate, moe_w_up, moe_w_down):
    h = x
    shared = None
    for layer in range(DEPTH):
        if layer < N_A_LAYERS:
            i = layer
            mix = gdn_mixer(h, gdn_w_in[i], gdn_conv_w[i], gdn_a_log[i], gdn_dt_bias[i], gdn_norm_w[i], gdn_w_o[i])
        else:
            i = layer - N_A_LAYERS
            mix = nsa_mixer(h, shared, nsa_w_in[i], nsa_w_o[i], rel_bias)
        h = layer_norm(DEEPNORM_ALPHA * h + mix, ln_mix_g[layer], ln_mix_b[layer])
        ffn = hier_moe(h, moe_w_grp[layer], moe_b_grp[layer], moe_w_rt[layer], moe_b_rt[layer],
                       moe_w_gate[layer], moe_w_up[layer], moe_w_down[layer])
        h = layer_norm(DEEPNORM_ALPHA * h + ffn, ln_ffn_g[layer], ln_ffn_b[layer])
        if layer == N_A_LAYERS - 1:
            shared = nsa_shared_kv(h, nsa_w_kv, cmp_k_pos, cmp_k_w1, cmp_k_w2, cmp_v_pos, cmp_v_w1, cmp_v_w2)
    return h
```

```python
from contextlib import ExitStack
import numpy as np
import concourse.bass as bass
import concourse.mybir as mybir

F32 = mybir.dt.float32
BF16 = mybir.dt.bfloat16
ALU = mybir.AluOpType
AF = mybir.ActivationFunctionType
AX = mybir.AxisListType

EPOCH = 30000
N_DMA_SEMS = 24


class Buf:
    __slots__ = ("name", "w", "r", "excl")

    def __init__(self, name="", excl=False):
        self.name = name
        self.w = None
        self.r = {}
        self.excl = excl


class Tile:
    def __init__(self, ap, buf=None, name=""):
        self.ap = ap
        self.buf = buf if buf is not None else Buf(name)

    def __getitem__(self, key):
        return Tile(self.ap[key], self.buf)

    def split(self, key, name=""):
        return Tile(self.ap[key], Buf(name))

    def bitcast(self, dt):
        return Tile(self.ap.bitcast(dt), self.buf)

    @property
    def shape(self):
        return self.ap.shape


def _ap(x):
    return x.ap if isinstance(x, Tile) else x


class Sched:
    def __init__(self, nc, stack):
        self.nc = nc
        self.stack = stack
        self.engs = {"pe": nc.tensor, "dve": nc.vector, "act": nc.scalar,
                     "pool": nc.gpsimd, "sp": nc.sync}
        self.prog = {e: [] for e in self.engs}
        self.cnt = {e: 0 for e in self.engs}
        self.esems = {e: [] for e in self.engs}
        self.waited = {e: {} for e in self.engs}
        self.dma_sems = [stack.enter_context(nc.semaphore(f"dq{i}")) for i in range(N_DMA_SEMS)]
        self.dma_uses = [0] * N_DMA_SEMS
        self.dma_rr = 0
        self.n_instr = 0
        self.same_engine_sync = True

    def sbuf(self, name, shape, dt, stack=None):
        self.uid = getattr(self, "uid", 0) + 1
        name = f"{name}_{self.uid}"
        t = (stack or self.stack).enter_context(self.nc.sbuf_tensor(name, list(shape), dt))
        return Tile(t[:], name=name)

    def psum(self, name, shape, dt, stack=None):
        t = (stack or self.stack).enter_context(self.nc.psum_tensor(name, list(shape), dt))
        return Tile(t[:], Buf(name, excl=True))

    def barrier(self):
        cnts = dict(self.cnt)
        uses = list(self.dma_uses)
        for e in self.engs:
            waits = []
            wd = self.waited[e]
            for f in self.engs:
                if f == e or cnts[f] == 0:
                    continue
                if wd.get(("eng", f), 0) >= cnts[f]:
                    continue
                wd[("eng", f)] = cnts[f]
                waits.append(self._esem(f, cnts[f]))
            for si, u in enumerate(uses):
                if u and wd.get(("dma", si), 0) < u * 16:
                    wd[("dma", si)] = u * 16
                    waits.append((self.dma_sems[si], u * 16))

            def run(E, waits=waits):
                for (ss, v) in waits:
                    E.wait_ge(ss, v)

            self.prog[e].append(run)

    def _esem(self, e, idx):
        k = (idx - 1) // EPOCH
        while len(self.esems[e]) <= k:
            self.esems[e].append(self.stack.enter_context(
                self.nc.semaphore(f"s_{e}_{len(self.esems[e])}")))
        return self.esems[e][k], (idx - 1) % EPOCH + 1

    def _collect(self, e, reads, writes):
        deps = set()
        for t in reads:
            b = t.buf
            if b.w is not None:
                deps.add(b.w)
            if b.excl:
                deps.update(b.r.values())
        for t in writes:
            b = t.buf
            if b.w is not None:
                deps.add(b.w)
            deps.update(b.r.values())
        waits = []
        wd = self.waited[e]
        best = {}
        for d in deps:
            key = (d[0], d[1]) if d[0] == "dma" else ("eng", d[1])
            if key not in best or d[2] > best[key][2]:
                best[key] = d
        for key, d in best.items():
            if d[0] == "eng":
                f, idx = d[1], d[2]
                if f == e and not (self.same_engine_sync and e != "pe"):
                    continue
                if wd.get(("eng", f), 0) >= idx:
                    continue
                wd[("eng", f)] = idx
                waits.append(self._esem(f, idx))
            else:
                s, v = d[1], d[2]
                if wd.get(("dma", s), 0) >= v:
                    continue
                wd[("dma", s)] = v
                waits.append((self.dma_sems[s], v))
        return waits

    def op(self, e, fn, reads=(), writes=()):
        reads = [t for t in reads if isinstance(t, Tile)]
        writes = [t for t in writes if isinstance(t, Tile)]
        waits = self._collect(e, reads, writes)
        self.cnt[e] += 1
        idx = self.cnt[e]
        sem, val = self._esem(e, idx)
        tok = ("eng", e, idx)

        def run(E, fn=fn, waits=waits, sem=sem):
            for (s, v) in waits:
                E.wait_ge(s, v)
            fn(E).then_inc(sem, 1)

        self.prog[e].append(run)
        self.n_instr += 1
        for t in writes:
            t.buf.w = tok
            t.buf.r = {}
        for t in reads:
            if t.buf.w is tok:
                continue
            if t.buf.excl:
                t.buf.w = tok
                t.buf.r = {}
            else:
                t.buf.r[("eng", e)] = tok
        return tok

    def dma(self, e, out, in_, **kw):
        reads = [in_] if isinstance(in_, Tile) else []
        writes = [out] if isinstance(out, Tile) else []
        waits = self._collect(e, reads, writes)
        s = self.dma_rr
        self.dma_rr = (self.dma_rr + 1) % N_DMA_SEMS
        prev = self.dma_uses[s] * 16
        self.dma_uses[s] += 1
        val = self.dma_uses[s] * 16
        wd = self.waited[e]
        if prev > 0 and wd.get(("dma", s), 0) < prev:
            wd[("dma", s)] = prev
            waits.append((self.dma_sems[s], prev))
        tok = ("dma", s, val)
        sem = self.dma_sems[s]
        oa, ia = _ap(out), _ap(in_)

        def run(E, waits=waits, sem=sem, oa=oa, ia=ia, kw=kw):
            for (ss, v) in waits:
                E.wait_ge(ss, v)
            E.dma_start(out=oa, in_=ia, **kw).then_inc(sem, 16)

        self.prog[e].append(run)
        self.n_instr += 1
        for t in writes:
            t.buf.w = tok
            t.buf.r = {}
        for t in reads:
            t.buf.r[("dma", s)] = tok
        return tok

    def wait_all_dma(self, e):
        uses = list(self.dma_uses)
        sems = self.dma_sems

        def run(E):
            for s, u in zip(sems, uses):
                if u:
                    E.wait_ge(s, u * 16)

        self.prog[e].append(run)

    def mm(self, out, lhsT, rhs, start=True, stop=True, **kw):
        return self.op("pe", lambda E: E.matmul(out.ap, lhsT.ap, rhs.ap, start=start, stop=stop, **kw),
                       reads=[lhsT, rhs] + ([] if start else [out]), writes=[out])

    def transpose(self, out, in_, ident):
        return self.op("pe", lambda E: E.transpose(out.ap, in_.ap, ident.ap),
                       reads=[in_, ident], writes=[out])

    def act(self, out, in_, func, bias=0.0, scale=1.0, accum_out=None, e="act"):
        r = [in_] + [x for x in (bias, scale) if isinstance(x, Tile)]
        w = [out] + ([accum_out] if accum_out is not None else [])
        kw = {}
        if accum_out is not None:
            kw["accum_out"] = accum_out.ap
        return self.op(e, lambda E: E.activation(out.ap, in_.ap, func, bias=_ap(bias), scale=_ap(scale), **kw),
                       reads=r, writes=w)

    def tt(self, out, in0, in1, op, e="dve"):
        return self.op(e, lambda E: E.tensor_tensor(out.ap, in0.ap, in1.ap, op),
                       reads=[in0, in1], writes=[out])

    def ts(self, out, in0, s1, op0, s2=None, op1=None, e="dve", accum_out=None):
        r = [in0] + [x for x in (s1, s2) if isinstance(x, Tile)]
        w = [out] + ([accum_out] if accum_out is not None else [])
        kw = {}
        if op1 is not None:
            kw["op1"] = op1
        if accum_out is not None:
            kw["accum_out"] = accum_out.ap
        return self.op(e, lambda E: E.tensor_scalar(out.ap, in0.ap, _ap(s1), _ap(s2) if s2 is not None else None, op0, **kw),
                       reads=r, writes=w)

    def stt(self, out, in0, scalar, in1, op0, op1, e="dve"):
        r = [in0, in1] + ([scalar] if isinstance(scalar, Tile) else [])
        return self.op(e, lambda E: E.scalar_tensor_tensor(out.ap, in0.ap, _ap(scalar), in1.ap, op0, op1),
                       reads=r, writes=[out])

    def copy(self, out, in_, e="dve"):
        if e == "act":
            return self.op(e, lambda E: E.copy(out.ap, in_.ap), reads=[in_], writes=[out])
        return self.op(e, lambda E: E.tensor_copy(out.ap, in_.ap), reads=[in_], writes=[out])

    def memset(self, out, val, e="dve"):
        return self.op(e, lambda E: E.memset(out.ap, val), writes=[out])

    def emit(self):
        nc = self.nc
        with nc.Block() as block:
            @block.tensor
            def _(E):
                for f in self.prog["pe"]:
                    f(E)

            @block.vector
            def _(E):
                for f in self.prog["dve"]:
                    f(E)

            @block.scalar
            def _(E):
                for f in self.prog["act"]:
                    f(E)

            @block.gpsimd
            def _(E):
                for f in self.prog["pool"]:
                    f(E)

            @block.sync
            def _(E):
                for f in self.prog["sp"]:
                    f(E)


import math
import os

T = 2048
D = 1024
NT = 16
NTG = 4
ALPHA = 4 ** 0.25
NEG = -30000.0


def t5_lo_thresholds():
    n = np.arange(0, 4096)
    nf = np.maximum(n, 1).astype(np.float32)
    large = 16 + (np.log(nf / np.float32(16)) / np.float32(math.log(128 / 16)) * np.float32(16)).astype(np.int32)
    large = np.minimum(large, 31)
    bucket = np.where(n < 16, n, large)
    return [int(np.argmax(bucket >= b)) for b in range(1, 32)]


class Pack:
    def __init__(self):
        self.parts = []
        self.off = {}
        self.n = 0

    def add(self, name, arr):
        arr = np.asarray(arr, dtype=np.float32).reshape(128, -1)
        self.off[name] = (self.n, self.n + arr.shape[1])
        self.n += arr.shape[1]
        self.parts.append(arr)

    def build(self):
        return np.ascontiguousarray(np.concatenate(self.parts, axis=1))


def rep(v):
    v = np.asarray(v, dtype=np.float32).reshape(1, -1)
    return np.repeat(v, 128, axis=0)


def col_chunks(v):
    v = np.asarray(v, dtype=np.float32)
    return v.reshape(-1, 128).T


def host_pack(inp):
    i = np.arange(128)
    cf = Pack()
    cf.add("ident", np.eye(128))
    cf.add("U", (i[:, None] <= i[None, :]))
    cf.add("NEGU", np.where(i[None, :] >= i[:, None], 0.0, -1e5))
    cf.add("SU", (i[None, :] > i[:, None]))
    cf.add("ones", np.ones((128, 128)))
    cw = inp["gdn_conv_w"][0]
    cf.add("convw", cw.reshape(4, 24, 128).transpose(2, 0, 1))
    cf.add("alog", rep(inp["gdn_a_log"][0]))
    cf.add("dtb", rep(inp["gdn_dt_bias"][0]))
    cf.add("normw", inp["gdn_norm_w"][0].reshape(128, 1))
    lng = np.stack([inp["ln_mix_g"][0], inp["ln_ffn_g"][0], inp["ln_mix_g"][1], inp["ln_ffn_g"][1]])
    lnb = np.stack([inp["ln_mix_b"][0], inp["ln_ffn_b"][0], inp["ln_mix_b"][1], inp["ln_ffn_b"][1]])
    cf.add("lng", lng.reshape(4, 8, 128).transpose(2, 0, 1))
    cf.add("lnb", lnb.reshape(4, 8, 128).transpose(2, 0, 1))
    rb = np.concatenate([inp["moe_b_grp"], inp["moe_b_rt"]], axis=1)
    cf.add("rbias", rep(rb.reshape(-1)))
    return cf


def host_weights(inp):
    w = {}
    win = inp["gdn_w_in"][0]
    w["gdn_w_in_r"] = np.ascontiguousarray(win[:, :4096].reshape(1024, 4, 8, 128).transpose(0, 2, 1, 3).reshape(1024, 8, 512))
    w["gdn_w_ab"] = np.ascontiguousarray(win[:, 4096:4112])
    w["gdn_w_o"] = np.ascontiguousarray(inp["gdn_w_o"][0])
    w["moe_w_r"] = np.ascontiguousarray(np.concatenate([inp["moe_w_grp"], inp["moe_w_rt"]], axis=2))
    w["moe_w_gate"] = inp["moe_w_gate"]
    w["moe_w_up"] = inp["moe_w_up"]
    w["moe_w_down"] = inp["moe_w_down"]
    return w


class Ctx:
    pass


def build_program(nc, nseq, stages="ABCI", dbg=None):
    dr = {}

    def dram_in(name, shape, dt=F32):
        dr[name] = nc.dram_tensor(name, list(shape), dt, kind="ExternalInput").ap()
        return dr[name]

    x_d = dram_in("x", [nseq, T, D])
    cf_host_n = build_program.cf_n
    cf_off = build_program.cf_off
    cf_d = dram_in("cf", [128, cf_host_n])
    w_in_r = dram_in("gdn_w_in_r", [1024, 8, 512])
    w_ab = dram_in("gdn_w_ab", [1024, 16])
    w_o_d = dram_in("gdn_w_o", [1024, 1024])
    moe_w_r = dram_in("moe_w_r", [2, 1024, 20])
    moe_w_gate = dram_in("moe_w_gate", [2, 16, 1024, 256])
    moe_w_up = dram_in("moe_w_up", [2, 16, 1024, 256])
    moe_w_down = dram_in("moe_w_down", [2, 16, 256, 1024])
    y_d = nc.dram_tensor("y", [nseq, T, D], F32, kind="ExternalOutput").ap()
    Cn = Ctx()
    Cn.cn_n = build_program.cn_n
    Cn.cn_off = build_program.cn_off
    nsa_declare(Cn, nc, dram_in)

    with ExitStack() as st:
        s = Sched(nc, st)
        C = Ctx()
        C.s = s
        cf = s.sbuf("cf_sb", [128, cf_host_n], F32)
        s.dma("sp", cf, cf_d)

        def cfs(name, a=None, b=None):
            o0, o1 = cf_off[name]
            if a is None:
                return cf[:, o0:o1]
            return cf[:, o0 + a:o0 + b]

        ident_f = cfs("ident")
        U_f = cfs("U")
        NEGU_f = cfs("NEGU")
        SU_f = cfs("SU")
        ones_f = cfs("ones")
        ident_b = s.sbuf("ident_b", [128, 128], BF16)
        ones_b = s.sbuf("ones_b", [128, 128], BF16)
        s.copy(ident_b, ident_f)
        s.copy(ones_b, ones_f)
        hT = s.sbuf("hT", [128, 8, T], F32)
        hbf = s.sbuf("hbf", [128, 8, T], BF16)
        R3 = s.sbuf("R3", [128, 8, T], BF16)
        PS = [s.psum(f"ps{i}", [128, 512], F32) for i in range(7)]
        PSB = s.psum("psb", [128, 1024], BF16)
        pbias = s.sbuf("pbias", [128, 2], F32)
        C.__dict__.update(locals())
        C.__dict__.update(Cn.__dict__)
        if "F" in stages:
            stage_nsa_setup(C)

        for seq in range(nseq):
            if "A" in stages:
                stage_load(C, seq)
            if "B" in stages:
                stage_gdn(C, seq)
            if "C" in stages:
                stage_proj_ln(C, seq, w_o_d, 0, R3)
            if "D" in stages:
                stage_moe(C, seq, 0)
            if "E" in stages:
                stage_kv(C, seq)
            if "F" in stages:
                stage_nsa(C, seq)
            elif "E" in stages:
                s.barrier()
                C.kv_stack.close()
            if "G" in stages:
                stage_proj_ln(C, seq, C.nsa_w_o, 2, R3)
            if "H" in stages:
                stage_moe(C, seq, 1)
            if "I" in stages:
                stage_store(C, seq)
        s.barrier()
        s.wait_all_dma("sp")
        s.emit()
        print("instructions:", s.n_instr, {e: s.cnt[e] for e in s.cnt})


def stage_load(C, seq):
    s = C.s
    with ExitStack() as st2:
        xin = [s.sbuf(f"xin{i}", [128, D], F32, stack=st2) for i in range(2)]
        for t in range(NT):
            xt = xin[t % 2]
            s.dma("sp", xt, C.x_d[seq, t * 128:(t + 1) * 128, :])
            for half in range(2):
                p = C.PS[(t * 2 + half) % 2]
                for c4 in range(4):
                    c = half * 4 + c4
                    s.transpose(p[:, c4 * 128:(c4 + 1) * 128], xt[:, c * 128:(c + 1) * 128], C.ident_f)
                src = Tile(p.ap.rearrange("p (c n) -> p c n", c=4), p.buf)
                s.copy(C.hT[:, half * 4:half * 4 + 4, t * 128:(t + 1) * 128], src, e="dve")
                s.copy(C.hbf[:, half * 4:half * 4 + 4, t * 128:(t + 1) * 128], src, e="act")
        s.barrier()


def stage_store(C, seq):
    s = C.s
    with ExitStack() as st2:
        yout = [s.sbuf(f"yout{i}", [128, D], F32, stack=st2) for i in range(2)]
        for t in range(NT):
            yt = yout[t % 2]
            for half in range(2):
                p = C.PS[(t * 2 + half) % 2]
                for c4 in range(4):
                    c = half * 4 + c4
                    s.transpose(p[:, c4 * 128:(c4 + 1) * 128], C.hT[:, c, t * 128:(t + 1) * 128], C.ident_f)
                s.copy(yt[:, half * 512:(half + 1) * 512], p, e="act" if half else "dve")
            s.dma("sp", C.y_d[seq, t * 128:(t + 1) * 128, :], yt)
        s.barrier()


def rsqrt_from_psum(C, out, ps, scale, eps, tmp):
    s = C.s
    s.act(tmp, ps, AF.Sqrt, bias=eps, scale=scale)
    s.op("dve", lambda E: E.reciprocal(out.ap, tmp.ap), reads=[tmp], writes=[out])


def stage_gdn(C, seq):
    s = C.s
    cfs = C.cfs
    hT, hbf, R3, PS, PSB = C.hT, C.hbf, C.R3, C.PS, C.PSB
    with ExitStack() as st2:
        def sb(name, shape, dt):
            return s.sbuf(name, shape, dt, stack=st2)

        wab = sb("wab", [128, 8, 16], F32)
        s.dma("sp", wab, C.w_ab.rearrange("(c p) n -> p c n", p=128))
        ab = sb("ab_tok", [128, NT, 16], F32)
        p = PS[0]
        for t in range(NT):
            for c in range(8):
                s.mm(p[:, t * 16:(t + 1) * 16], hT[:, c, t * 128:(t + 1) * 128], wab[:, c, :],
                     start=(c == 0), stop=(c == 7))
        s.copy(ab, Tile(p.ap[:, 0:256].rearrange("p (t n) -> p t n", t=NT), p.buf))

        def bc8(name):
            t_ = cfs(name)
            return Tile(t_.ap.unsqueeze(1).to_broadcast([128, NT, 8]), t_.buf)

        g = sb("g_tok", [128, NT, 8], F32)
        t1 = sb("t1", [128, NT, 8], F32)
        t2 = sb("t2", [128, NT, 8], F32)
        negA = sb("negA", [128, 8], F32)
        s.act(negA, cfs("alog"), AF.Exp)
        s.ts(negA, negA, -1.0, ALU.mult)
        s.tt(t1, ab[:, :, 0:8], bc8("dtb"), ALU.add)
        s.ts(t2, t1, 60.0, ALU.min)
        s.act(t2, t2, AF.Exp)
        s.act(t2, t2, AF.Ln, bias=1.0)
        s.tt(t2, t2, t1, ALU.max)
        negA_b = Tile(negA.ap.unsqueeze(1).to_broadcast([128, NT, 8]), negA.buf)
        s.tt(g, t2, negA_b, ALU.mult)
        beta = sb("beta", [128, NT, 8], F32)
        s.act(beta, ab[:, :, 8:16], AF.Sigmoid)
        negbeta = sb("negbeta", [128, NT, 8], F32)
        s.ts(negbeta, beta, -1.0, ALU.mult)
        gc = sb("gc", [128, NT, 8], F32)
        glast = sb("glast", [128, NT, 8], F32)
        p = PS[1]
        for t in range(NT):
            s.mm(p[:, t * 8:(t + 1) * 8], C.U_f, g[:, t, :])
        s.copy(gc, Tile(p.ap[:, 0:128].rearrange("p (t n) -> p t n", t=NT), p.buf))
        p = PS[2]
        for t in range(NT):
            s.mm(p[:, t * 8:(t + 1) * 8], C.ones_f, g[:, t, :])
        s.copy(glast, Tile(p.ap[:, 0:128].rearrange("p (t n) -> p t n", t=NT), p.buf))
        negegc = sb("negegc", [128, NT, 8], F32)
        s.act(negegc, gc, AF.Exp)
        s.ts(negegc, negegc, -1.0, ALU.mult)
        d_b = sb("d_b", [128, NT, 8], F32)
        s.act(d_b, glast, AF.Exp)
        bdecl = sb("bdecl", [128, NT, 8], F32)
        s.tt(bdecl, glast, gc, ALU.subtract)
        s.act(bdecl, bdecl, AF.Exp)
        s.tt(bdecl, bdecl, beta, ALU.mult)

        wq = sb("wq", [128, 8, 384], BF16)
        wz = [sb(f"wz{i}", [128, 8, 128], BF16) for i in range(2)]
        seg = [sb(f"cinseg{i}", [128, 515], F32) for i in range(4)]
        s.memset(seg[0][:, 0:3], 0.0)
        acc = sb("acc", [128, T], F32)
        accseg = [acc.split((slice(None), slice(i * 512, (i + 1) * 512)), f"accseg{i}") for i in range(4)]
        sqb = sb("sqb", [128, T], BF16)
        VT = sqb
        sq2 = [sb(f"sq2_{i}", [128, 512], BF16) for i in range(2)]
        rsT = sb("rsT", [128, 48], F32)
        QT = sb("QT", [128, T], BF16)
        KT = sb("KT", [128, T], BF16)
        ztmp = sb("ztmp", [128, 512], BF16)
        Ktok = sb("Ktok", [128, NT, 128], BF16)
        Vtok = sb("Vtok", [128, NT, 128], BF16)
        Nb = [sb(f"Nb{i}", [128, 4, 128], BF16) for i in range(2)]
        qkT = [sb(f"qkT{i}", [128, 4, 128], BF16) for i in range(2)]
        qdT = [sb(f"qdT{i}", [128, 4, 128], BF16) for i in range(2)]
        A1 = sb("A1", [128, 4, 128], F32)
        A2 = sb("A2", [128, 4, 128], F32)
        A3 = sb("A3", [128, 4, 128], F32)
        Pm = [sb(f"Pm{i}", [128, 4, 128], BF16) for i in range(2)]
        PTm = [sb(f"PTm{i}", [128, 4, 128], BF16) for i in range(2)]
        Nbt = [sb(f"Nbt{i}", [128, 4, 128], BF16) for i in range(2)]
        S = sb("S", [128, 128], F32)
        Sb = sb("Sb", [128, 128], BF16)
        Y = sb("Y", [128, 128], BF16)
        vnew = sb("vnew", [128, 128], BF16)
        vdec = sb("vdec", [128, 128], BF16)

        def flat(t_):
            return Tile(t_.ap.rearrange("p a b -> p (a b)"), t_.buf)

        def v4(t_):
            return Tile(t_.ap.rearrange("p (a b) -> p a b", a=4), t_.buf)

        def bc_mid(t_):
            return Tile(t_.ap.unsqueeze(1).to_broadcast([128, 4, 128]), t_.buf)

        def bc_last(t_):
            return Tile(t_.ap.to_broadcast([128, 4, 128]), t_.buf)

        rsq, rsq2 = flat(A1), flat(A2)

        print("gdn sbuf bytes remaining", C.nc.sbuf_bytes_remaining)

        w_r = C.w_in_r.rearrange("(c p) h f -> p c h f", p=128)

        def load_wq(h):
            s.dma("pool", wq, w_r[:, :, h, 0:384])

        def load_wz(h):
            s.dma("pool", wz[h % 2], w_r[:, :, h, 384:512])

        def phase1(h, grp):
            n0 = grp * 4
            b = grp % 2
            tsl = slice(n0 * 128, (n0 + 4) * 128)
            gsl = g[:, n0:n0 + 4, h:h + 1]
            s.tt(A1, bc_mid(C.U_f), bc_last(gsl), ALU.mult)
            s.mm(PS[2], C.ones_f, flat(A1))
            for k in range(4):
                sl = slice((n0 + k) * 128, (n0 + k + 1) * 128)
                s.mm(PS[3][:, k * 128:(k + 1) * 128], KT[:, sl], KT[:, sl])
            for k in range(4):
                sl = slice((n0 + k) * 128, (n0 + k + 1) * 128)
                s.mm(PS[4][:, k * 128:(k + 1) * 128], KT[:, sl], QT[:, sl])
            yield
            s.tt(A1, v4(PS[2]), bc_last(gc[:, n0:n0 + 4, h:h + 1]), ALU.subtract)
            s.tt(A1, A1, bc_mid(C.NEGU_f), ALU.add)
            s.act(A2, v4(PS[2]), AF.Exp)
            s.act(A1, A1, AF.Exp)
            s.tt(qdT[b], v4(QT[:, tsl]), A2, ALU.mult)
            s.tt(A2, A1, bc_mid(C.SU_f), ALU.mult)
            s.tt(qkT[b], v4(PS[4]), A1, ALU.mult)
            s.tt(A3, v4(PS[3]), bc_last(negbeta[:, n0:n0 + 4, h:h + 1]), ALU.mult)
            s.tt(Pm[0], A3, A2, ALU.mult)
            for k in range(4):
                s.transpose(PSB[:, k * 128:(k + 1) * 128], Pm[0][:, k, :], C.ident_b)
            yield
            s.copy(PTm[0], v4(PSB[:, 0:512]), e="act")
            s.tt(A3, Pm[0], bc_mid(C.ident_f), ALU.add)
            s.copy(Nbt[0], A3, e="act")
            cur = 0
            for j in range(1, 7):
                nxt = 1 - cur
                if j < 6:
                    for k in range(4):
                        s.mm(PS[3][:, k * 128:(k + 1) * 128], PTm[cur][:, k, :], Pm[cur][:, k, :])
                for k in range(4):
                    s.mm(PS[4][:, k * 128:(k + 1) * 128], Pm[cur][:, k, :], PTm[cur][:, k, :])
                yield
                if j < 6:
                    s.copy(Pm[nxt], v4(PS[3]), e="act")
                s.copy(PTm[nxt], v4(PS[4]), e="dve")
                for k in range(4):
                    s.mm(PS[5][:, k * 128:(k + 1) * 128], PTm[nxt][:, k, :], Nbt[cur][:, k, :])
                yield
                s.tt(A3, A3, v4(PS[5]), ALU.add)
                if j < 6:
                    s.copy(Nbt[nxt], A3, e="act")
                else:
                    s.copy(Nb[b], A3, e="act")
                cur = nxt

        def chain(h, grp):
            b = grp % 2
            for k in range(4):
                n = grp * 4 + k
                sl = slice(n * 128, (n + 1) * 128)
                s.mm(PS[6][:, 0:128], KT[:, sl], Sb)
                yield
                s.stt(Y, PS[6][:, 0:128], negegc[:, n, h:h + 1], Vtok[:, n, :], ALU.mult, ALU.add)
                s.mm(PS[6][:, 128:256], Nb[b][:, k, :], Y)
                yield
                s.ts(vnew, PS[6][:, 128:256], beta[:, n, h:h + 1], ALU.mult)
                s.ts(vdec, PS[6][:, 128:256], bdecl[:, n, h:h + 1], ALU.mult)
                s.mm(PS[0][:, 0:128], Sb, qdT[b][:, k, :], start=True, stop=False)
                s.mm(PS[0][:, 0:128], vnew, qkT[b][:, k, :], start=False, stop=True)
                s.mm(PS[1][:, 0:128], Ktok[:, n, :], vdec)
                yield
                s.copy(accseg[n // 4][:, (n % 4) * 128:(n % 4 + 1) * 128], PS[0][:, 0:128], e="act")
                s.stt(S, S, d_b[:, n, h:h + 1], PS[1][:, 0:128], ALU.mult, ALU.add)
                s.copy(Sb, S, e="act")

        def run_gens(*gens):
            gens = list(gens)
            while gens:
                for gi in list(gens):
                    try:
                        next(gi)
                    except StopIteration:
                        gens.remove(gi)

        load_wq(0)
        load_wz(0)
        for h in range(8):
            w = wq
            steps = [(part, tg) for part in range(3) for tg in range(NTG)]

            def stageA(i):
                part, tg = steps[i]
                pp = PS[i % 2]
                for c in range(8):
                    s.mm(pp, w[:, c, part * 128:(part + 1) * 128], hbf[:, c, tg * 512:(tg + 1) * 512],
                         start=(c == 0), stop=(c == 7))
                s.copy(seg[tg][:, 3:515], pp, e="act")
                if tg + 1 < NTG:
                    s.copy(seg[tg + 1][:, 0:3], seg[tg][:, 512:515], e="pool")

            def stageB(i):
                part, tg = steps[i]
                a = accseg[tg]
                cwi = lambda j: C.cfs("convw", j * 24 + part * 8 + h, j * 24 + part * 8 + h + 1)
                s.ts(a, seg[tg][:, 0:512], cwi(0), ALU.mult)
                for j in range(1, 4):
                    s.stt(a, seg[tg][:, j:j + 512], cwi(j), a, ALU.mult, ALU.add)
                s.act(a, a, AF.Silu)
                if part < 2:
                    s.act(sq2[i % 2], a, AF.Square)
                else:
                    s.copy(VT[:, tg * 512:(tg + 1) * 512], a, e="act")

            def bc_rs(col0):
                t_ = rsT[:, col0:col0 + 4]
                return Tile(t_.ap.unsqueeze(2).to_broadcast([128, 4, 128]), t_.buf)

            def bcast_rs(col0):
                s.tt(A1, bc_mid(C.ident_f), bc_rs(col0), ALU.mult)
                s.mm(PS[2], C.ones_f, flat(A1))

            def stageC(i):
                part, tg = steps[i]
                if part == 2:
                    return
                for k in range(4):
                    col = part * 16 + tg * 4 + k
                    s.mm(PS[3][:, col:col + 1], sq2[i % 2][:, k * 128:(k + 1) * 128], C.ones_b[:, 0:1])
                if tg == NTG - 1:
                    cs = slice(part * 16, part * 16 + 16)
                    s.act(rsT[:, cs], PS[3][:, cs], AF.Sqrt, bias=1e-6)
                    s.op("dve", lambda E, cs=cs: E.reciprocal(rsT.ap[:, cs], rsT.ap[:, cs]), reads=[rsT], writes=[rsT])
                    for t2 in range(NTG):
                        tsl = slice(t2 * 512, (t2 + 1) * 512)
                        bcast_rs(part * 16 + t2 * 4)
                        if part == 0:
                            s.stt(QT[:, tsl], accseg[t2], 128 ** -0.5, PS[2], ALU.mult, ALU.mult)
                        else:
                            s.tt(KT[:, tsl], accseg[t2], PS[2], ALU.mult)

            def tok_major(src, dst):
                for t4 in range(4):
                    for k in range(4):
                        t = t4 * 4 + k
                        s.transpose(PSB[:, k * 128:(k + 1) * 128], src[:, t * 128:(t + 1) * 128], C.ident_b)
                    s.copy(dst[:, t4 * 4:(t4 + 1) * 4, :], v4(PSB[:, 0:512]), e="act")

            for i in range(len(steps) + 2):
                if i < len(steps):
                    stageA(i)
                if 0 <= i - 2 < len(steps):
                    stageC(i - 2)
                if 0 <= i - 1 < len(steps):
                    stageB(i - 1)
                if i == 11:
                    tok_major(KT, Ktok)
            tok_major(VT, Vtok)
            if h + 1 < 8:
                load_wq(h + 1)
                load_wz(h + 1)
            s.memset(S, 0.0)
            s.memset(Sb, 0.0)
            run_gens(phase1(h, 0))
            for grp in range(4):
                if grp + 1 < 4:
                    run_gens(phase1(h, grp + 1), chain(h, grp))
                else:
                    run_gens(chain(h, grp))
            for tg in range(NTG):
                s.act(sq2[tg % 2], accseg[tg], AF.Square)
                for k in range(4):
                    col = 32 + tg * 4 + k
                    s.mm(PS[3][:, col:col + 1], sq2[tg % 2][:, k * 128:(k + 1) * 128], C.ones_b[:, 0:1])
            s.act(rsT[:, 32:48], PS[3][:, 32:48], AF.Sqrt, bias=1e-6, scale=1.0 / 128)
            s.op("dve", lambda E: E.reciprocal(rsT.ap[:, 32:48], rsT.ap[:, 32:48]), reads=[rsT], writes=[rsT])
            for tg in range(NTG):
                tsl = slice(tg * 512, (tg + 1) * 512)
                pp = PS[tg % 2]
                for c in range(8):
                    s.mm(pp, wz[h % 2][:, c, :], hbf[:, c, tsl], start=(c == 0), stop=(c == 7))
                s.act(ztmp, pp, AF.Silu)
                bcast_rs(32 + tg * 4)
                s.tt(rsq2, accseg[tg], PS[2], ALU.mult)
                s.stt(R3[:, h, tsl], rsq2, C.cfs("normw"), ztmp, ALU.mult, ALU.mult)
        s.barrier()


def layer_norm_tg(C, st2, tg, k, scratch):
    s = C.s
    hT, hbf, PS = C.hT, C.hbf, C.PS
    sqs, mean, msq, rstd, tmp = scratch
    tsl = slice(tg * 512, (tg + 1) * 512)
    for c in range(8):
        s.copy(hbf[:, c, tsl], hT[:, c, tsl], e="act")
        s.act(sqs[:, c, :], hT[:, c, tsl], AF.Square)
    for c in range(8):
        s.mm(PS[4], C.ones_b, hbf[:, c, tsl], start=(c == 0), stop=(c == 7))
    for c in range(8):
        s.mm(PS[5], C.ones_b, sqs[:, c, :], start=(c == 0), stop=(c == 7))
    s.ts(mean, PS[4], 1.0 / D, ALU.mult)
    s.tt(msq, mean, mean, ALU.mult)
    s.stt(msq, PS[5], 1.0 / D, msq, ALU.mult, ALU.subtract)
    s.act(tmp, msq, AF.Sqrt, bias=1e-5)
    s.op("dve", lambda E: E.reciprocal(rstd.ap, tmp.ap), reads=[tmp], writes=[rstd])
    for c in range(8):
        gcol = C.cfs("lng", k * 8 + c, k * 8 + c + 1)
        bcol = C.cfs("lnb", k * 8 + c, k * 8 + c + 1)
        s.tt(tmp, hT[:, c, tsl], mean, ALU.subtract)
        s.tt(tmp, tmp, rstd, ALU.mult, e="pool")
        s.ts(hT[:, c, tsl], tmp, gcol, ALU.mult, bcol, ALU.add)
        s.copy(hbf[:, c, tsl], hT[:, c, tsl], e="act")


def ln_scratch(C, st2):
    s = C.s
    return (s.sbuf("ln_sqs", [128, 8, 512], BF16, stack=st2),
            s.sbuf("ln_mean", [128, 512], F32, stack=st2),
            s.sbuf("ln_msq", [128, 512], F32, stack=st2),
            s.sbuf("ln_rstd", [128, 512], F32, stack=st2),
            s.sbuf("ln_tmp", [128, 512], F32, stack=st2))


def stage_proj_ln(C, seq, w_d, k, src):
    s = C.s
    hT, PS = C.hT, C.PS
    with ExitStack() as st2:
        wo = s.sbuf("wo", [128, 8, 1024], BF16, stack=st2)
        s.dma("pool", wo, w_d.rearrange("(c p) n -> p c n", p=128))
        scratch = ln_scratch(C, st2)
        def proj(tg):
            tsl = slice(tg * 512, (tg + 1) * 512)
            for c in range(8):
                pp = PS[c % 4]
                for kc in range(8):
                    s.mm(pp, wo[:, kc, c * 128:(c + 1) * 128], src[:, kc, tsl], start=(kc == 0), stop=(kc == 7))
                s.stt(hT[:, c, tsl], hT[:, c, tsl], ALPHA, pp, ALU.mult, ALU.add)
        proj(0)
        for tg in range(NTG):
            if tg + 1 < NTG:
                proj(tg + 1)
            layer_norm_tg(C, st2, tg, k, scratch)
        s.barrier()


def stage_moe(C, seq, layer):
    s = C.s
    hT, hbf, R3, PS = C.hT, C.hbf, C.R3, C.PS
    hdn = R3
    with ExitStack() as st2:
        def sb(name, shape, dt):
            return s.sbuf(name, shape, dt, stack=st2)

        wr = sb("wr", [128, 8, 20], F32)
        s.dma("sp", wr, C.moe_w_r[layer].rearrange("(c p) n -> p c n", p=128))
        lg = sb("lg", [128, NT, 20], F32)
        p = PS[0]
        for t in range(NT):
            for c in range(8):
                s.mm(p[:, t * 20:(t + 1) * 20], hT[:, c, t * 128:(t + 1) * 128], wr[:, c, :],
                     start=(c == 0), stop=(c == 7))
        rb = C.cfs("rbias", layer * 20, layer * 20 + 20)
        s.tt(lg, Tile(p.ap[:, 0:NT * 20].rearrange("p (t n) -> p t n", t=NT), p.buf),
             Tile(rb.ap.unsqueeze(1).to_broadcast([128, NT, 20]), rb.buf), ALU.add)

        def red(out, in_, op):
            s.op("dve", lambda E: E.tensor_reduce(out.ap, in_.ap, AX.X, op), reads=[in_], writes=[out])

        def bl(t_, n):
            return Tile(t_.ap.unsqueeze(2).to_broadcast([128, t_.ap.shape[1], n]), t_.buf)

        lgv = lg[:, :, 0:4]
        le = sb("le", [128, NT * 4, 4], F32)
        s.copy(Tile(le.ap.rearrange("p (t g) i -> p t (g i)", t=NT), le.buf), lg[:, :, 4:20])
        mg = sb("mg", [128, NT], F32)
        eg = sb("eg", [128, NT, 4], F32)
        sg = sb("sg", [128, NT], F32)
        oh = sb("oh", [128, NT, 4], F32)
        red(mg, lgv, ALU.max)
        s.tt(eg, lgv, bl(mg, 4), ALU.subtract)
        s.tt(oh, lgv, bl(mg, 4), ALU.is_ge)
        s.act(eg, eg, AF.Exp)
        red(sg, eg, ALU.add)
        s.op("dve", lambda E: E.reciprocal(sg.ap, sg.ap), reads=[sg], writes=[sg])
        s.tt(oh, oh, bl(sg, 4), ALU.mult)
        m1 = sb("m1", [128, NT * 4], F32)
        Ee = sb("Ee", [128, NT * 4, 4], F32)
        t1 = sb("mt1", [128, NT * 4, 4], F32)
        red(m1, le, ALU.max)
        s.tt(Ee, le, bl(m1, 4), ALU.subtract)
        s.tt(t1, le, bl(m1, 4), ALU.is_ge)
        s.act(Ee, Ee, AF.Exp)
        s.stt(t1, t1, -1e30, le, ALU.mult, ALU.add)
        m2 = sb("m2", [128, NT * 4], F32)
        red(m2, t1, ALU.max)
        s.tt(t1, le, bl(m2, 4), ALU.is_ge)
        s.tt(Ee, Ee, t1, ALU.mult)
        red(m2, Ee, ALU.add)
        s.op("dve", lambda E: E.reciprocal(m2.ap, m2.ap), reads=[m2], writes=[m2])
        ohf = Tile(oh.ap.rearrange("p t g -> p (t g)"), oh.buf)
        s.tt(m2, m2, ohf, ALU.mult)
        gate = sb("gate", [128, NT * 4, 4], F32)
        s.tt(gate, Ee, bl(m2, 4), ALU.mult)
        gate_v = Tile(gate.ap.rearrange("p (t g) i -> p t (g i)", t=NT), gate.buf)
        gateT = sb("gateT", [16, T], BF16)
        for tg in range(NTG):
            for k in range(4):
                t = tg * 4 + k
                s.transpose(PS[1][0:16, k * 128:(k + 1) * 128], gate_v[:, t, :], C.ident_f)
            s.copy(gateT[:, tg * 512:(tg + 1) * 512], PS[1][0:16, :], e="act")
        selE = sb("selE", [16, 16, 128], BF16)
        idv = C.ident_f[0:16, 0:16]
        s.copy(selE, Tile(idv.ap.unsqueeze(2).to_broadcast([16, 16, 128]), idv.buf))

        for c in range(8):
            s.ts(hT[:, c, :], hT[:, c, :], ALPHA, ALU.mult, e=("pool" if c % 2 else "dve"))
        wgu = [sb(f"wgu{i}", [128, 2, 8, 256], BF16) for i in range(2)]
        wd = sb("wd", [128, 4, 2, 1024], BF16)
        sgt = sb("sgt", [128, 512], F32)
        tmu = sb("tmu", [128, 512], F32)
        gbanks = [PS[4], C.PSB.bitcast(F32)]
        for e in range(16):
            wb = wgu[e % 2]
            s.dma("pool", wb[:, 0], C.moe_w_gate[layer, e].rearrange("(c p) f -> p c f", p=128))
            s.dma("pool", wb[:, 1], C.moe_w_up[layer, e].rearrange("(c p) f -> p c f", p=128))
            s.dma("pool", wd[:, e % 4], C.moe_w_down[layer, e].rearrange("(c p) n -> p c n", p=128))
            for tg in range(NTG):
                tsl = slice(tg * 512, (tg + 1) * 512)
                gbk = gbanks[tg % 2]
                s.mm(gbk, selE[:, e, :], gateT[:, tsl])
                for fc in range(2):
                    for c in range(8):
                        s.mm(PS[2 * fc], wb[:, 0, c, fc * 128:(fc + 1) * 128], hbf[:, c, tsl],
                             start=(c == 0), stop=(c == 7))
                    for c in range(8):
                        s.mm(PS[2 * fc + 1], wb[:, 1, c, fc * 128:(fc + 1) * 128], hbf[:, c, tsl],
                             start=(c == 0), stop=(c == 7))
                    s.act(sgt, PS[2 * fc], AF.Silu)
                    s.tt(tmu, sgt, PS[2 * fc + 1], ALU.mult)
                    s.tt(hdn[:, (e % 4) * 2 + fc, tsl], tmu, gbk, ALU.mult)
            if e % 4 == 3:
                for tg in range(NTG):
                    tsl = slice(tg * 512, (tg + 1) * 512)
                    for c in range(8):
                        pp = PS[5 + c % 2]
                        for k in range(8):
                            s.mm(pp, wd[:, k // 2, k % 2, c * 128:(c + 1) * 128], hdn[:, k, tsl],
                                 start=(k == 0), stop=(k == 7))
                        s.tt(hT[:, c, tsl], hT[:, c, tsl], pp, ALU.add)
        s.barrier()
    with ExitStack() as st2:
        scratch = ln_scratch(C, st2)
        for tg in range(NTG):
            layer_norm_tg(C, st2, tg, 2 * layer + 1, scratch)
        s.barrier()


def host_pack_nsa(inp):
    cn = Pack()
    i = np.arange(128)
    Dn = np.concatenate([(128 * j + i[None, :] - i[:, None]) for j in range(2)], axis=1)
    cn.add("Dnear", Dn)
    Dc = np.full((128, 128), -1.0)
    for m in range(24):
        Dc[m] = i - 16 * (m - 16) - 31
    Dc[24] = 1000.0
    cn.add("Dcmp", Dc)
    Wc = np.zeros((128, 16, 128))
    for c in range(16):
        for n in range(128):
            n1 = n - 8 * c
            m = n1 + 16 if -16 <= n1 <= 7 else (24 if n1 < -16 else 25)
            if n == 127:
                m = 25
            Wc[m, c, n] = 1.0
    cn.add("Wc", Wc)
    WM4 = np.where(i[:, None] > i[None, :], 0.0, NEG)
    cn.add("WM4x4", np.tile(WM4, (1, 4)))
    Ee = np.zeros((128, 16, 128))
    for t in range(16):
        for k in range(128):
            Ee[2 * t + (k >= 64), t, k] = 1.0
    cn.add("Eexp", Ee)
    OV = np.zeros((128, 33))
    for n in range(127):
        for j in range(32):
            if 16 * n < 64 * j + 64 and 16 * n + 31 >= 64 * j:
                OV[n, j] = 1.0
        OV[n, 32] = 1.0
    cn.add("OVL", OV)
    valid = np.zeros((128, 16, 32)); addm = np.zeros((128, 16, 32))
    for c in range(16):
        for ii in range(128):
            jq = 2 * c + (ii >= 64)
            for j in range(32):
                forced = (j == 0) or (j == jq) or (j == jq - 1)
                if forced:
                    addm[ii, c, j] = 1e9
                elif j > jq:
                    addm[ii, c, j] = -1e9
                else:
                    valid[ii, c, j] = 1.0
    cn.add("valid", valid)
    cn.add("addm", addm)
    SP = np.zeros((128, 24, 128))
    for g in range(4):
        for p in range(2):
            for br in range(3):
                for slot in range(2):
                    r = (g * 4 + p * 2 + slot) * 3 + br
                    SP[r, (g * 2 + p) * 3 + br, slot * 64:(slot + 1) * 64] = 1.0
    cn.add("SelPair", SP)
    cn.add("relb", rep(inp["rel_bias"].reshape(-1)))
    return cn


def host_weights_nsa(inp):
    w = {}
    wkv = inp["nsa_w_kv"].reshape(1024, 6, 4, 64)
    w["nsa_w_raw"] = np.ascontiguousarray(wkv[:, [0, 1]].reshape(1024, 512))
    kd = wkv[:, [2, 4]]
    w["nsa_w_kdup"] = np.ascontiguousarray(np.repeat(kd[:, :, :, None, :], 2, axis=3).reshape(1024, 2, 512))
    w["nsa_w_vtok"] = np.ascontiguousarray(wkv[:, [3, 5]].reshape(1024, 2, 256))
    w["nsa_w_in"] = np.ascontiguousarray(inp["nsa_w_in"][0])
    w["nsa_w_o"] = np.ascontiguousarray(inp["nsa_w_o"][0])
    w1 = np.stack([inp["cmp_k_w1"], inp["cmp_v_w1"]]).reshape(2, 32, 64, 128).transpose(0, 2, 1, 3)
    w["cmp_w1dup"] = np.ascontiguousarray(np.concatenate([w1, w1], axis=1))
    w["cmp_w2k_dup"] = np.ascontiguousarray(np.concatenate([inp["cmp_k_w2"], inp["cmp_k_w2"]], axis=1))
    w["cmp_w2v"] = np.ascontiguousarray(inp["cmp_v_w2"])
    pT = np.stack([inp["cmp_k_pos"].T, inp["cmp_v_pos"].T])
    w["cmp_posT"] = np.ascontiguousarray(np.concatenate([pT, pT], axis=1))
    return w


def nsa_declare(C, nc, dram_in):
    C.cn_d = dram_in("cn", [128, C.cn_n])
    C.nsa_w_raw = dram_in("nsa_w_raw", [1024, 512])
    C.nsa_w_kdup = dram_in("nsa_w_kdup", [1024, 2, 512])
    C.nsa_w_vtok = dram_in("nsa_w_vtok", [1024, 2, 256])
    C.nsa_w_in = dram_in("nsa_w_in", [1024, 1072])
    C.nsa_w_o = dram_in("nsa_w_o", [1024, 1024])
    C.cmp_w1dup = dram_in("cmp_w1dup", [2, 128, 32, 128])
    C.cmp_w2k_dup = dram_in("cmp_w2k_dup", [128, 128])
    C.cmp_w2v = dram_in("cmp_w2v", [128, 64])
    C.cmp_posT = dram_in("cmp_posT", [2, 128, 32])
    C.bt_scr = nc.dram_tensor("bt_scr", [128, 4, 2, 512], BF16, kind="Internal").ap()
    C.pat_scr = nc.dram_tensor("pat_scr", [32, 4, 512], BF16, kind="Internal").ap()


def cn_slice(C, name):
    o0, o1 = C.cn_off[name]
    return C.cn_d[:, o0:o1]


SLOT_HEADS = (0, 2, 1, 3)


def stage_nsa_setup(C):
    s = C.s
    PS = C.PS
    los = t5_lo_thresholds()
    with ExitStack() as st2:
        def sb(name, shape, dt):
            return s.sbuf(name, shape, dt, stack=st2)
        relb = sb("relb", [128, 512], F32)
        s.dma("sp", relb, cn_slice(C, "relb"))
        negD = sb("negD", [128, 31 * 16], F32)
        s.tt(negD, relb[:, 0:31 * 16], relb[:, 16:512], ALU.subtract)
        for which in ("near", "cmp"):
            W = 256 if which == "near" else 128
            NP = 128 if which == "near" else 26
            Dt_ = sb("D" + which, [128, W], F32)
            s.dma("sp", Dt_, cn_slice(C, "Dnear" if which == "near" else "Dcmp"))
            accs_all = sb("accs" + which, [128, 16, W], F32)
            acch = [accs_all.split((slice(0, NP), h, slice(None)), f"acc{which}{h}") for h in range(16)]
            Lts = [sb(f"Lt{which}{i}", [128, W], F32) for i in range(2)]
            for h in range(16):
                s.memset(acch[h], 0.0, e=("pool" if h % 2 else "dve"))
            for b in range(1, 33):
                Lt = Lts[b % 2]
                if b < 32:
                    s.ts(Lt[0:NP], Dt_[0:NP], float(los[b - 1]), ALU.is_lt)
                else:
                    s.ts(Lt[0:NP], Dt_[0:NP], 0.0, ALU.is_lt)
                for h in range(16):
                    if b < 32:
                        s.stt(acch[h], Lt[0:NP], negD[0:NP, (b - 1) * 16 + h:(b - 1) * 16 + h + 1], acch[h], ALU.mult, ALU.add)
                    else:
                        s.stt(acch[h], Lt[0:NP], NEG, acch[h], ALU.mult, ALU.add)
            if which == "near":
                bt = sb("bt_b", [128, 4, 2, 512], BF16)
                for g in range(4):
                    for sl_, hh in enumerate(SLOT_HEADS):
                        for j in range(2):
                            s.copy(bt[:, g, j, sl_ * 128:(sl_ + 1) * 128], acch[4 * g + hh][:, j * 128:(j + 1) * 128],
                                   e=("act" if j else "dve"))
                s.dma("sp", C.bt_scr, bt)
            else:
                pt = sb("pat_b", [32, 4, 512], BF16)
                s.memset(pt, 0.0)
                for g in range(4):
                    for sl_, hh in enumerate(SLOT_HEADS):
                        s.copy(pt[0:26, g, sl_ * 128:(sl_ + 1) * 128], acch[4 * g + hh])
                s.dma("sp", C.pat_scr, pt)
        w1 = sb("w1s", [128, 32, 128], BF16)
        posT = sb("posTs", [128, 2, 32], BF16)
        s.dma("pool", posT, C.cmp_posT.rearrange("k p l -> p k l"))
        for kv in range(2):
            s.dma("pool", w1, C.cmp_w1dup[kv])
            for l in range(32):
                s.mm(PS[0][:, kv:kv + 1], w1[0:64, l, :], posT[0:64, kv, l:l + 1], start=(l == 0), stop=(l == 31))
        s.copy(C.pbias, PS[0][:, 0:2])
        s.barrier()


def stage_kv(C, seq):
    s = C.s
    hbf, R3, PS = C.hbf, C.R3, C.PS
    C.kv_stack = ExitStack()
    st = C.kv_stack

    def sbp(name, shape, dt):
        return s.sbuf(name, shape, dt, stack=st)
    C.ksT2 = sbp("ksT2", [128, 4, T], BF16)
    C.kwT2 = sbp("kwT2", [128, 4, T], BF16)
    C.vs_tok = sbp("vs_tok", [128, NT, 256], BF16)
    C.vw_tok = sbp("vw_tok", [128, NT, 256], BF16)
    C.sgT = sbp("sgT", [48, T], BF16)
    C.kcT2 = sbp("kcT2", [128, 4, 128], BF16)
    C.vc = sbp("vc", [128, 4, 64], BF16)
    raw = R3
    with ExitStack() as st2:
        def sb(name, shape, dt):
            return s.sbuf(name, shape, dt, stack=st2)
        wk = sb("wkv_a", [128, 8, 512], BF16)
        s.dma("pool", wk, C.nsa_w_raw.rearrange("(c p) n -> p c n", p=128))
        for ch in range(4):
            for tg in range(NTG):
                tsl = slice(tg * 512, (tg + 1) * 512)
                pp = PS[tg % 2]
                for c in range(8):
                    s.mm(pp, wk[:, c, ch * 128:(ch + 1) * 128], hbf[:, c, tsl], start=(c == 0), stop=(c == 7))
                s.copy(raw[:, ch, tsl], pp, e=("act" if tg % 2 else "dve"))
        w1 = sb("w1c", [128, 32, 128], BF16)
        w2k = sb("w2k", [128, 128], BF16)
        w2v = sb("w2v", [128, 64], BF16)
        hid = sb("hidc", [128, 128], BF16)
        s.dma("pool", w2k, C.cmp_w2k_dup)
        s.dma("pool", w2v, C.cmp_w2v)
        s.memset(C.kcT2, 0.0)
        s.memset(C.vc, 0.0)
        for kv in range(2):
            s.dma("pool", w1, C.cmp_w1dup[kv])
            for g in range(4):
                po = (g % 2) * 64
                ch = kv * 2 + g // 2
                for l in range(32):
                    rhs = Tile(raw.ap[po:po + 64, ch, l:l + 16 * 126 + 1:16], raw.buf)
                    s.mm(PS[2][:, 0:127], w1[po:po + 64, l, :], rhs, start=(l == 0), stop=(l == 31))
                s.act(hid[:, 0:127], PS[2][:, 0:127], AF.Silu, bias=C.pbias[:, kv:kv + 1])
                if kv == 0:
                    s.mm(PS[3][:, 0:127], w2k, hid[:, 0:127])
                    s.copy(C.kcT2[:, g, 0:127], PS[3][:, 0:127])
                else:
                    s.mm(PS[3][0:127, 0:64], hid[:, 0:127], w2v)
                    s.copy(C.vc[0:127, g, :], PS[3][0:127, 0:64])
        for part, dst in ((0, C.ksT2), (1, C.kwT2)):
            s.dma("pool", wk, C.nsa_w_kdup.rearrange("(c p) k n -> p c k n", p=128)[:, :, part, :])
            for g in range(4):
                for tg in range(NTG):
                    tsl = slice(tg * 512, (tg + 1) * 512)
                    pp = PS[tg % 2]
                    for c in range(8):
                        s.mm(pp, wk[:, c, g * 128:(g + 1) * 128], hbf[:, c, tsl], start=(c == 0), stop=(c == 7))
                    s.copy(dst[:, g, tsl], pp, e=("act" if tg % 2 else "dve"))
        wv = Tile(wk.ap[:, :, 0:256], wk.buf)
        for part, dst in ((0, C.vs_tok), (1, C.vw_tok)):
            s.dma("pool", wv, C.nsa_w_vtok.rearrange("(c p) k n -> p c k n", p=128)[:, :, part, :])
            for t in range(NT):
                pp = PS[t % 2]
                for c in range(8):
                    s.mm(pp[:, 0:256], hbf[:, c, t * 128:(t + 1) * 128], wv[:, c, :], start=(c == 0), stop=(c == 7))
                s.copy(dst[:, t, :], pp[:, 0:256], e=("act" if t % 2 else "dve"))
        s.barrier()
    with ExitStack() as st2:
        wi = s.sbuf("wi", [128, 8, 1072], BF16, stack=st2)
        s.dma("pool", wi, C.nsa_w_in.rearrange("(c p) n -> p c n", p=128))
        for ch in range(9):
            M = 128 if ch < 8 else 48
            for tg in range(NTG):
                tsl = slice(tg * 512, (tg + 1) * 512)
                pp = PS[tg % 2]
                for c in range(8):
                    s.mm(pp[0:M, :], wi[:, c, ch * 128:ch * 128 + M], hbf[:, c, tsl], start=(c == 0), stop=(c == 7))
                if ch < 8:
                    s.act(R3[:, ch, tsl], pp, AF.Copy, scale=0.125)
                else:
                    s.act(C.sgT[:, tsl], pp[0:48, :], AF.Sigmoid)
        s.barrier()


def stage_nsa(C, seq):
    s = C.s
    PS, PSB, R3 = C.PS, C.PSB, C.R3
    S_ps = [PS[0], PS[1]]
    O_ps = {"cmp": PS[2], "sel": PS[3], "win": PS[4]}
    G2, IMP = PS[5], PS[6]
    arena = C.hbf.ap.rearrange("p a b -> p (a b)")
    off = [0]

    def carve(np_, ncols, dt=BF16):
        n2 = ncols * (2 if dt == F32 else 1)
        a = arena[0:np_, off[0]:off[0] + n2]
        off[0] += n2
        if dt == F32:
            a = a.bitcast(F32)
        return Tile(a, Buf())
    BT = carve(128, 4 * 2 * 512)
    Pat = carve(32, 4 * 512)
    Wc = carve(32, 16 * 128)
    WM4 = carve(128, 512)
    Eexp = carve(32, 16 * 128)
    SelP = carve(48, 24 * 128)
    OVL = carve(128, 33 + 1, F32)
    valid = carve(128, 512, F32)
    addm = carve(128, 512, F32)
    assert off[0] <= 16384, off[0]
    s.dma("sp", BT, C.bt_scr.rearrange("p g j n -> p (g j n)"))
    s.dma("sp", Pat, C.pat_scr.rearrange("p g n -> p (g n)"))
    s.dma("pool", Wc, cn_slice(C, "Wc")[0:32, :])
    s.dma("pool", WM4, cn_slice(C, "WM4x4"))
    s.dma("pool", Eexp, cn_slice(C, "Eexp")[0:32, :])
    s.dma("pool", SelP, cn_slice(C, "SelPair")[0:48, :])
    s.dma("sp", OVL[:, 0:33], cn_slice(C, "OVL"))
    s.dma("sp", valid, cn_slice(C, "valid"))
    s.dma("sp", addm, cn_slice(C, "addm"))
    BTv = Tile(BT.ap.rearrange("p (g j n) -> p g j n", g=4, j=2), BT.buf)
    Patv = Tile(Pat.ap.rearrange("p (g n) -> p g n", g=4), Pat.buf)
    Wcv = Tile(Wc.ap.rearrange("p (c n) -> p c n", c=16), Wc.buf)
    Eev = Tile(Eexp.ap.rearrange("p (t k) -> p t k", t=16), Eexp.buf)
    SelPv = Tile(SelP.ap.rearrange("p (a n) -> p a n", a=24), SelP.buf)
    validv = Tile(valid.ap.rearrange("p (c j) -> p c j", c=16), valid.buf)
    addmv = Tile(addm.ap.rearrange("p (c j) -> p c j", c=16), addm.buf)
    qblk = [[Tile(R3.ap[:, 2 * g:2 * g + 2, c * 128:(c + 1) * 128], Buf()) for g in range(4)] for c in range(16)]
    with ExitStack() as st2:
        def sb(name, shape, dt):
            return s.sbuf(name, shape, dt, stack=st2)
        Pb = [sb(f"Pb{i}", [128, 512], BF16) for i in range(3)]
        Pf = sb("Pf", [128, 512], F32)
        rd = sb("rd", [128, 256], F32)
        fac = sb("fac", [128, 256], F32)
        con = sb("con", [128, 256], F32)
        oacc = sb("oacc", [128, 256], F32)
        rec4 = sb("rec4", [128, 4], F32)
        impn = sb("impn", [128, 32], F32)
        imp2 = sb("imp2", [128, 32], F32)
        m8a = sb("m8a", [128, 8], F32)
        m8b = sb("m8b", [128, 8], F32)
        nsel = sb("nsel", [128, 32], BF16)
        nselT = sb("nselT", [32, 4, 128], BF16)
        pbi = [0]

        qA = [sb(f"qA{i}", [128, 2, 128], BF16) for i in range(2)]
        qB = [sb(f"qB{i}", [128, 2, 128], BF16) for i in range(2)]
        for i in range(2):
            s.memset(qA[i], 0.0)
            s.memset(qB[i], 0.0)
        qcur = [None, None]

        def qk(Sps, kT2, g, t, c, first_start):
            ksl = slice(t * 128, (t + 1) * 128)
            s.mm(Sps[:, 0:256], kT2[:, g, ksl], qcur[0], start=first_start, stop=False, skip_group_check=True)
            s.mm(Sps[:, 256:512], kT2[:, g, ksl], qcur[1], start=False, stop=True, skip_group_check=True)

        def pv(O, P, vtile, np_, first):
            for half in range(2):
                rows = slice(half * 64, half * 64 + 64)
                pc = slice(half * 256, half * 256 + 256)
                s.mm(O[rows, 0:256], vtile, P[0:np_, pc], start=first, stop=False, skip_group_check=True)
                s.mm(O[rows, 256:512], C.ones_b[0:np_, 0:64], P[0:np_, pc], start=False, stop=False, skip_group_check=True)

        def combine(br_i, key, c, g, oacc, gt):
            O = O_ps[key]
            s.ts(rd, O[:, 256:512], 1e-30, ALU.max)
            s.op("dve", lambda E: E.reciprocal(rd.ap, rd.ap), reads=[rd], writes=[rd])
            s.tt(fac, rd, gt[:, br_i * 256:(br_i + 1) * 256], ALU.mult)
            if br_i == 0:
                s.tt(oacc, O[:, 0:256], fac, ALU.mult)
            else:
                s.tt(con, O[:, 0:256], fac, ALU.mult)
                s.tt(oacc, oacc, con, ALU.add, e="pool")

        blocks = [(c, g) for c in range(16) for g in range(4)]
        nsel2 = [nsel, sb("nsel_b", [128, 32], BF16)]
        nselT2 = [nselT, sb("nselT_b", [32, 4, 128], BF16)]
        oacc2 = [oacc, sb("oacc_b", [128, 256], F32)]
        Pc = sb("Pc", [128, 512], BF16)

        gate3 = [sb(f"gate3_{i}", [128, 768], F32) for i in range(2)]

        def prep(b):
            c, g = blocks[b]
            q = qblk[c][g]
            s.copy(qA[b % 2][0:64], q[0:64], e="act")
            s.copy(qB[b % 2][64:128], q[64:128], e="act")
            sg = C.sgT[:, c * 128:(c + 1) * 128]
            for br in (1, 2):
                for p_ in range(2):
                    s.mm(G2[:, (br - 1) * 256 + p_ * 128:(br - 1) * 256 + (p_ + 1) * 128],
                         SelPv[:, (g * 2 + p_) * 3 + br, :], sg)
            for p_ in range(2):
                s.mm(IMP[:, 256 + p_ * 128:256 + (p_ + 1) * 128], SelPv[:, (g * 2 + p_) * 3 + 0, :], sg)
            s.copy(gate3[b % 2][:, 256:768], G2, e="act")
            s.copy(gate3[b % 2][:, 0:256], IMP[:, 256:512], e="act")

        def qk2(Sps, kT2, g, t, qa, qb, first_start):
            ksl = slice(t * 128, (t + 1) * 128)
            s.mm(Sps[:, 0:256], kT2[:, g, ksl], qa, start=first_start, stop=False, skip_group_check=True)
            s.mm(Sps[:, 256:512], kT2[:, g, ksl], qb, start=False, stop=True, skip_group_check=True)

        def cmpA(b):
            c, g = blocks[b]
            qa, qb = qA[b % 2], qB[b % 2]
            Sps, P = S_ps[0], Pc
            s.mm(Sps, Wcv[0:26, c, :], Patv[0:26, g, :], start=True, stop=False, skip_group_check=True)
            s.mm(Sps[:, 0:256], C.kcT2[:, g, :], qa, start=False, stop=False, skip_group_check=True)
            s.mm(Sps[:, 256:512], C.kcT2[:, g, :], qb, start=False, stop=True, skip_group_check=True)
            s.act(Pf, Sps, AF.Exp)
            s.copy(P, Pf)
            pv(O_ps["cmp"], P, C.vc[:, g, :], 128, True)
            for sl_ in range(4):
                s.mm(IMP[:, sl_ * 33:(sl_ + 1) * 33], Pf[:, sl_ * 128:(sl_ + 1) * 128], OVL[:, 0:33])
            impv = Tile(IMP.ap[:, 0:132].rearrange("p (s n) -> p s n", s=4), IMP.buf)
            s.ts(rec4, impv[:, :, 32], 1e-30, ALU.max)
            s.op("dve", lambda E: E.reciprocal(rec4.ap, rec4.ap), reads=[rec4], writes=[rec4])
            s.ts(impn, impv[:, 0, 0:32], rec4[:, 0:1], ALU.mult)
            for sl_ in range(1, 4):
                s.stt(impn, impv[:, sl_, 0:32], rec4[:, sl_:sl_ + 1], impn, ALU.mult, ALU.add)
            s.tt(impn, impn, validv[:, c, :], ALU.mult)
            s.tt(impn, impn, addmv[:, c, :], ALU.add)
            s.op("dve", lambda E: E.max(out=m8a.ap, in_=impn.ap), reads=[impn], writes=[m8a])
            s.op("dve", lambda E: E.match_replace(out=imp2.ap, in_to_replace=m8a.ap, in_values=impn.ap, imm_value=-3e9),
                 reads=[m8a, impn], writes=[imp2])
            s.op("dve", lambda E: E.max(out=m8b.ap, in_=imp2.ap), reads=[imp2], writes=[m8b])
            s.ts(imp2, impn, m8b[:, 7:8], ALU.is_ge)
            s.ts(nsel2[b % 2], imp2, -NEG, ALU.mult, NEG, ALU.add)

        def cmpB(b):
            c, g = blocks[b]
            s.transpose(PSB[0:32, 0:128], nsel2[b % 2], C.ident_b)
            s.copy(nselT2[b % 2], Tile(PSB.ap[0:32, 0:128].unsqueeze(1).to_broadcast([32, 4, 128]), PSB.buf), e="act")
            combine(0, "cmp", c, g, oacc2[b % 2], gate3[b % 2])

        def block_tiles(b):
            c, g = blocks[b]
            qa, qb = qA[b % 2], qB[b % 2]
            nT = nselT2[b % 2]
            nT_f = Tile(nT.ap.rearrange("p a b -> p (a b)"), nT.buf)
            tiles = []
            t0 = max(0, c - 4)
            for t in range(t0, c + 1):
                j = c - t

                def sc(Sps, t=t, j=j):
                    if j <= 1:
                        s.mm(Sps, C.ident_b, BTv[:, g, j, :], start=True, stop=False, skip_group_check=True)
                        qk2(Sps, C.kwT2, g, t, qa, qb, False)
                    elif j == 4:
                        s.mm(Sps, C.ident_b, WM4, start=True, stop=False, skip_group_check=True)
                        qk2(Sps, C.kwT2, g, t, qa, qb, False)
                    else:
                        qk2(Sps, C.kwT2, g, t, qa, qb, True)
                tiles.append((sc, "win", C.vw_tok[:, t, g * 64:(g + 1) * 64], t == t0))
            for t in range(c + 1):
                j = c - t

                def sc(Sps, t=t, j=j):
                    s.mm(Sps, Eev[0:32, t, :], nT_f, start=True, stop=False, skip_group_check=True)
                    qk2(Sps, C.ksT2, g, t, qa, qb, False)
                    if j <= 1:
                        s.mm(Sps, C.ident_b, BTv[:, g, j, :], start=False, stop=True, skip_group_check=True)
                tiles.append((sc, "sel", C.vs_tok[:, t, g * 64:(g + 1) * 64], t == 0))
            return tiles

        def run_block(b):
            c, g = blocks[b]
            tiles = block_tiles(b)
            n = len(tiles)
            alloc = [(S_ps[i % 2], Pb[i % 3]) for i in range(n)]
            tiles[0][0](alloc[0][0])
            for i in range(n):
                if i + 1 < n:
                    tiles[i + 1][0](alloc[i + 1][0])
                Sps, P = alloc[i]
                s.act(P, Sps, AF.Exp)
                pv(O_ps[tiles[i][1]], P, tiles[i][2], 128, tiles[i][3])
                if i == 0 and b + 1 < len(blocks):
                    prep(b + 1)
                    cmpA(b + 1)
            combine(2, "win", c, g, oacc2[b % 2], gate3[b % 2])
            combine(1, "sel", c, g, oacc2[b % 2], gate3[b % 2])
            oa = oacc2[b % 2]
            s.copy(qblk[c][g], Tile(oa.ap.rearrange("p (a b) -> p a b", a=2), oa.buf), e="pool")

        prep(0)
        cmpA(0)
        cmpB(0)
        for b in range(len(blocks)):
            run_block(b)
            if b + 1 < len(blocks):
                cmpB(b + 1)
        s.barrier()
    C.kv_stack.close()


def kernel(**inputs):
    from concourse.bass_utils import run_bass_kernel_spmd
    inp = {k: np.asarray(v) for k, v in inputs.items()}
    x = inp["x"]
    ncores = 8
    nseq = x.shape[0] // ncores
    cf = host_pack(inp)
    cn = host_pack_nsa(inp)
    build_program.cf_n = cf.n
    build_program.cf_off = cf.off
    build_program.cn_n = cn.n
    build_program.cn_off = cn.off
    w = host_weights(inp)
    w.update(host_weights_nsa(inp))
    w["cf"] = cf.build()
    w["cn"] = cn.build()
    w = {k: np.ascontiguousarray(v, dtype=np.float32) for k, v in w.items()}
    nc = bass.Bass("TRN2", target_bir_lowering=False)
    build_program(nc, nseq, "ABCDEFGHI")
    maps = []
    for i in range(ncores):
        m = dict(w)
        m["x"] = np.ascontiguousarray(x[i * nseq:(i + 1) * nseq], dtype=np.float32)
        maps.append(m)
    res = run_bass_kernel_spmd(nc, maps, core_ids=list(range(ncores)))
    return np.concatenate([np.asarray(res.results[i]["y"]) for i in range(ncores)], axis=0).astype(np.float32)
```

```python
from contextlib import ExitStack
import numpy as np
import concourse.bass as bass
import concourse.mybir as mybir

F32 = mybir.dt.float32
BF16 = mybir.dt.bfloat16
ALU = mybir.AluOpType
AF = mybir.ActivationFunctionType
AX = mybir.AxisListType

EPOCH = 30000
N_DMA_SEMS = 24


class Buf:
    __slots__ = ("name", "w", "r", "excl")

    def __init__(self, name="", excl=False):
        self.name = name
        self.w = None
        self.r = {}
        self.excl = excl


class Tile:
    def __init__(self, ap, buf=None, name=""):
        self.ap = ap
        self.buf = buf if buf is not None else Buf(name)

    def __getitem__(self, key):
        return Tile(self.ap[key], self.buf)

    def split(self, key, name=""):
        return Tile(self.ap[key], Buf(name))

    def bitcast(self, dt):
        return Tile(self.ap.bitcast(dt), self.buf)

    @property
    def shape(self):
        return self.ap.shape


def _ap(x):
    return x.ap if isinstance(x, Tile) else x


class Sched:
    def __init__(self, nc, stack):
        self.nc = nc
        self.stack = stack
        self.engs = {"pe": nc.tensor, "dve": nc.vector, "act": nc.scalar,
                     "pool": nc.gpsimd, "sp": nc.sync}
        self.prog = {e: [] for e in self.engs}
        self.cnt = {e: 0 for e in self.engs}
        self.esems = {e: [] for e in self.engs}
        self.waited = {e: {} for e in self.engs}
        self.dma_sems = [stack.enter_context(nc.semaphore(f"dq{i}")) for i in range(N_DMA_SEMS)]
        self.dma_uses = [0] * N_DMA_SEMS
        self.dma_rr = 0
        self.n_instr = 0
        self.same_engine_sync = True

    def sbuf(self, name, shape, dt, stack=None):
        self.uid = getattr(self, "uid", 0) + 1
        name = f"{name}_{self.uid}"
        t = (stack or self.stack).enter_context(self.nc.sbuf_tensor(name, list(shape), dt))
        return Tile(t[:], name=name)

    def psum(self, name, shape, dt, stack=None):
        t = (stack or self.stack).enter_context(self.nc.psum_tensor(name, list(shape), dt))
        return Tile(t[:], Buf(name, excl=True))

    def barrier(self):
        cnts = dict(self.cnt)
        uses = list(self.dma_uses)
        for e in self.engs:
            waits = []
            wd = self.waited[e]
            for f in self.engs:
                if f == e or cnts[f] == 0:
                    continue
                if wd.get(("eng", f), 0) >= cnts[f]:
                    continue
                wd[("eng", f)] = cnts[f]
                waits.append(self._esem(f, cnts[f]))
            for si, u in enumerate(uses):
                if u and wd.get(("dma", si), 0) < u * 16:
                    wd[("dma", si)] = u * 16
                    waits.append((self.dma_sems[si], u * 16))

            def run(E, waits=waits):
                for (ss, v) in waits:
                    E.wait_ge(ss, v)

            self.prog[e].append(run)

    def _esem(self, e, idx):
        k = (idx - 1) // EPOCH
        while len(self.esems[e]) <= k:
            self.esems[e].append(self.stack.enter_context(
                self.nc.semaphore(f"s_{e}_{len(self.esems[e])}")))
        return self.esems[e][k], (idx - 1) % EPOCH + 1

    def _collect(self, e, reads, writes):
        deps = set()
        for t in reads:
            b = t.buf
            if b.w is not None:
                deps.add(b.w)
            if b.excl:
                deps.update(b.r.values())
        for t in writes:
            b = t.buf
            if b.w is not None:
                deps.add(b.w)
            deps.update(b.r.values())
        waits = []
        wd = self.waited[e]
        best = {}
        for d in deps:
            key = (d[0], d[1]) if d[0] == "dma" else ("eng", d[1])
            if key not in best or d[2] > best[key][2]:
                best[key] = d
        for key, d in best.items():
            if d[0] == "eng":
                f, idx = d[1], d[2]
                if f == e and not (self.same_engine_sync and e != "pe"):
                    continue
                if wd.get(("eng", f), 0) >= idx:
                    continue
                wd[("eng", f)] = idx
                waits.append(self._esem(f, idx))
            else:
                s, v = d[1], d[2]
                if wd.get(("dma", s), 0) >= v:
                    continue
                wd[("dma", s)] = v
                waits.append((self.dma_sems[s], v))
        return waits

    def op(self, e, fn, reads=(), writes=()):
        reads = [t for t in reads if isinstance(t, Tile)]
        writes = [t for t in writes if isinstance(t, Tile)]
        waits = self._collect(e, reads, writes)
        self.cnt[e] += 1
        idx = self.cnt[e]
        sem, val = self._esem(e, idx)
        tok = ("eng", e, idx)

        def run(E, fn=fn, waits=waits, sem=sem):
            for (s, v) in waits:
                E.wait_ge(s, v)
            fn(E).then_inc(sem, 1)

        self.prog[e].append(run)
        self.n_instr += 1
        for t in writes:
            t.buf.w = tok
            t.buf.r = {}
        for t in reads:
            if t.buf.w is tok:
                continue
            if t.buf.excl:
                t.buf.w = tok
                t.buf.r = {}
            else:
                t.buf.r[("eng", e)] = tok
        return tok

    def dma(self, e, out, in_, **kw):
        reads = [in_] if isinstance(in_, Tile) else []
        writes = [out] if isinstance(out, Tile) else []
        waits = self._collect(e, reads, writes)
        half = N_DMA_SEMS // 2
        if e == "pool":
            self.dma_rr_sw = (getattr(self, "dma_rr_sw", -1) + 1) % half
            s = half + self.dma_rr_sw
        else:
            self.dma_rr = (self.dma_rr + 1) % half
            s = self.dma_rr
        prev = self.dma_uses[s] * 16
        self.dma_uses[s] += 1
        val = self.dma_uses[s] * 16
        wd = self.waited[e]
        if prev > 0 and wd.get(("dma", s), 0) < prev:
            wd[("dma", s)] = prev
            waits.append((self.dma_sems[s], prev))
        tok = ("dma", s, val)
        sem = self.dma_sems[s]
        oa, ia = _ap(out), _ap(in_)

        def run(E, waits=waits, sem=sem, oa=oa, ia=ia, kw=kw):
            for (ss, v) in waits:
                E.wait_ge(ss, v)
            E.dma_start(out=oa, in_=ia, **kw).then_inc(sem, 16)

        self.prog[e].append(run)
        self.n_instr += 1
        for t in writes:
            t.buf.w = tok
            t.buf.r = {}
        for t in reads:
            t.buf.r[("dma", s)] = tok
        return tok

    def wait_all_dma(self, e):
        uses = list(self.dma_uses)
        sems = self.dma_sems

        def run(E):
            for s, u in zip(sems, uses):
                if u:
                    E.wait_ge(s, u * 16)

        self.prog[e].append(run)

    def mm(self, out, lhsT, rhs, start=True, stop=True, **kw):
        return self.op("pe", lambda E: E.matmul(out.ap, lhsT.ap, rhs.ap, start=start, stop=stop, **kw),
                       reads=[lhsT, rhs] + ([] if start else [out]), writes=[out])

    def transpose(self, out, in_, ident):
        return self.op("pe", lambda E: E.transpose(out.ap, in_.ap, ident.ap),
                       reads=[in_, ident], writes=[out])

    def act(self, out, in_, func, bias=0.0, scale=1.0, accum_out=None, e="act"):
        r = [in_] + [x for x in (bias, scale) if isinstance(x, Tile)]
        w = [out] + ([accum_out] if accum_out is not None else [])
        kw = {}
        if accum_out is not None:
            kw["accum_out"] = accum_out.ap
        return self.op(e, lambda E: E.activation(out.ap, in_.ap, func, bias=_ap(bias), scale=_ap(scale), **kw),
                       reads=r, writes=w)

    def tt(self, out, in0, in1, op, e="dve"):
        return self.op(e, lambda E: E.tensor_tensor(out.ap, in0.ap, in1.ap, op),
                       reads=[in0, in1], writes=[out])

    def ts(self, out, in0, s1, op0, s2=None, op1=None, e="dve", accum_out=None):
        r = [in0] + [x for x in (s1, s2) if isinstance(x, Tile)]
        w = [out] + ([accum_out] if accum_out is not None else [])
        kw = {}
        if op1 is not None:
            kw["op1"] = op1
        if accum_out is not None:
            kw["accum_out"] = accum_out.ap
        return self.op(e, lambda E: E.tensor_scalar(out.ap, in0.ap, _ap(s1), _ap(s2) if s2 is not None else None, op0, **kw),
                       reads=r, writes=w)

    def stt(self, out, in0, scalar, in1, op0, op1, e="dve"):
        r = [in0, in1] + ([scalar] if isinstance(scalar, Tile) else [])
        return self.op(e, lambda E: E.scalar_tensor_tensor(out.ap, in0.ap, _ap(scalar), in1.ap, op0, op1),
                       reads=r, writes=[out])

    def copy(self, out, in_, e="dve"):
        if e == "act":
            return self.op(e, lambda E: E.copy(out.ap, in_.ap), reads=[in_], writes=[out])
        return self.op(e, lambda E: E.tensor_copy(out.ap, in_.ap), reads=[in_], writes=[out])

    def memset(self, out, val, e="dve"):
        return self.op(e, lambda E: E.memset(out.ap, val), writes=[out])

    def emit(self):
        nc = self.nc
        with nc.Block() as block:
            @block.tensor
            def _(E):
                for f in self.prog["pe"]:
                    f(E)

            @block.vector
            def _(E):
                for f in self.prog["dve"]:
                    f(E)

            @block.scalar
            def _(E):
                for f in self.prog["act"]:
                    f(E)

            @block.gpsimd
            def _(E):
                for f in self.prog["pool"]:
                    f(E)

            @block.sync
            def _(E):
                for f in self.prog["sp"]:
                    f(E)


import math
import os

T = 2048
D = 1024
NT = 16
NTG = 4
ALPHA = 4 ** 0.25
NEG = -30000.0


def t5_lo_thresholds():
    n = np.arange(0, 4096)
    nf = np.maximum(n, 1).astype(np.float32)
    large = 16 + (np.log(nf / np.float32(16)) / np.float32(math.log(128 / 16)) * np.float32(16)).astype(np.int32)
    large = np.minimum(large, 31)
    bucket = np.where(n < 16, n, large)
    return [int(np.argmax(bucket >= b)) for b in range(1, 32)]


class Pack:
    def __init__(self):
        self.parts = []
        self.off = {}
        self.n = 0

    def add(self, name, arr):
        arr = np.asarray(arr, dtype=np.float32).reshape(128, -1)
        self.off[name] = (self.n, self.n + arr.shape[1])
        self.n += arr.shape[1]
        self.parts.append(arr)

    def build(self):
        return np.ascontiguousarray(np.concatenate(self.parts, axis=1))


def rep(v):
    v = np.asarray(v, dtype=np.float32).reshape(1, -1)
    return np.repeat(v, 128, axis=0)


def col_chunks(v):
    v = np.asarray(v, dtype=np.float32)
    return v.reshape(-1, 128).T


def host_pack(inp):
    i = np.arange(128)
    cf = Pack()
    cf.add("ident", np.eye(128))
    cf.add("U", (i[:, None] <= i[None, :]))
    cf.add("NEGU", np.where(i[None, :] >= i[:, None], 0.0, -1e5))
    cf.add("SU", (i[None, :] > i[:, None]))
    cf.add("ones", np.ones((128, 128)))
    cw = inp["gdn_conv_w"][0]
    cf.add("convw", cw.reshape(4, 24, 128).transpose(2, 0, 1))
    cf.add("alog", rep(inp["gdn_a_log"][0]))
    cf.add("dtb", rep(inp["gdn_dt_bias"][0]))
    cf.add("normw", inp["gdn_norm_w"][0].reshape(128, 1))
    lng = np.stack([inp["ln_mix_g"][0], inp["ln_ffn_g"][0], inp["ln_mix_g"][1], inp["ln_ffn_g"][1]])
    lnb = np.stack([inp["ln_mix_b"][0], inp["ln_ffn_b"][0], inp["ln_mix_b"][1], inp["ln_ffn_b"][1]])
    cf.add("lng", lng.reshape(4, 8, 128).transpose(2, 0, 1))
    cf.add("lnb", lnb.reshape(4, 8, 128).transpose(2, 0, 1))
    rb = np.concatenate([inp["moe_b_grp"], inp["moe_b_rt"]], axis=1)
    cf.add("rbias", rep(rb.reshape(-1)))
    return cf


def host_weights(inp):
    w = {}
    win = inp["gdn_w_in"][0]
    w["gdn_w_in_r"] = np.ascontiguousarray(win[:, :4096].reshape(1024, 4, 8, 128).transpose(0, 2, 1, 3).reshape(1024, 8, 512))
    w["gdn_w_ab"] = np.ascontiguousarray(win[:, 4096:4112])
    w["gdn_w_o"] = np.ascontiguousarray(inp["gdn_w_o"][0])
    w["moe_w_r"] = np.ascontiguousarray(np.concatenate([inp["moe_w_grp"], inp["moe_w_rt"]], axis=2))
    w["moe_w_gate"] = inp["moe_w_gate"]
    w["moe_w_up"] = inp["moe_w_up"]
    w["moe_w_down"] = inp["moe_w_down"]
    return w


class Ctx:
    pass


def build_program(nc, nseq, stages="ABCI", dbg=None):
    dr = {}

    def dram_in(name, shape, dt=F32):
        dr[name] = nc.dram_tensor(name, list(shape), dt, kind="ExternalInput").ap()
        return dr[name]

    x_d = dram_in("x", [nseq, T, D])
    cf_host_n = build_program.cf_n
    cf_off = build_program.cf_off
    cf_d = dram_in("cf", [128, cf_host_n])
    w_in_r = dram_in("gdn_w_in_r", [1024, 8, 512])
    w_ab = dram_in("gdn_w_ab", [1024, 16])
    w_o_d = dram_in("gdn_w_o", [1024, 1024])
    moe_w_r = dram_in("moe_w_r", [2, 1024, 20])
    moe_w_gate = dram_in("moe_w_gate", [2, 16, 1024, 256])
    moe_w_up = dram_in("moe_w_up", [2, 16, 1024, 256])
    moe_w_down = dram_in("moe_w_down", [2, 16, 256, 1024])
    y_d = nc.dram_tensor("y", [nseq, T, D], F32, kind="ExternalOutput").ap()
    Cn = Ctx()
    Cn.cn_n = build_program.cn_n
    Cn.cn_off = build_program.cn_off
    nsa_declare(Cn, nc, dram_in)

    with ExitStack() as st:
        s = Sched(nc, st)
        C = Ctx()
        C.s = s
        cf = s.sbuf("cf_sb", [128, cf_host_n], F32)
        s.dma("sp", cf, cf_d)

        def cfs(name, a=None, b=None):
            o0, o1 = cf_off[name]
            if a is None:
                return cf[:, o0:o1]
            return cf[:, o0 + a:o0 + b]

        ident_f = cfs("ident")
        U_f = cfs("U")
        NEGU_f = cfs("NEGU")
        SU_f = cfs("SU")
        ones_f = cfs("ones")
        ident_b = s.sbuf("ident_b", [128, 128], BF16)
        ones_b = s.sbuf("ones_b", [128, 128], BF16)
        s.copy(ident_b, ident_f)
        s.copy(ones_b, ones_f)
        hT = s.sbuf("hT", [128, 8, T], F32)
        hbf = s.sbuf("hbf", [128, 8, T], BF16)
        R3 = s.sbuf("R3", [128, 8, T], BF16)
        PS = [s.psum(f"ps{i}", [128, 512], F32) for i in range(7)]
        PSB = s.psum("psb", [128, 1024], BF16)
        pbias = s.sbuf("pbias", [128, 2], F32)
        C.__dict__.update(locals())
        C.__dict__.update(Cn.__dict__)
        if "F" in stages:
            stage_nsa_setup(C)

        for seq in range(nseq):
            if "A" in stages:
                stage_load(C, seq)
            if "B" in stages:
                stage_gdn(C, seq)
            if "C" in stages:
                stage_proj_ln(C, seq, w_o_d, 0, R3)
            if "D" in stages:
                stage_moe(C, seq, 0)
            if "E" in stages:
                stage_kv(C, seq)
            if "F" in stages:
                stage_nsa(C, seq)
            elif "E" in stages:
                s.barrier()
                C.kv_stack.close()
            if "G" in stages:
                stage_proj_ln(C, seq, C.nsa_w_o, 2, R3)
            if "H" in stages:
                stage_moe(C, seq, 1)
            if "I" in stages:
                stage_store(C, seq)
        s.barrier()
        s.wait_all_dma("sp")
        s.emit()
        print("instructions:", s.n_instr, {e: s.cnt[e] for e in s.cnt})


def stage_load(C, seq):
    s = C.s
    with ExitStack() as st2:
        xin = [s.sbuf(f"xin{i}", [128, D], F32, stack=st2) for i in range(2)]
        for t in range(NT):
            xt = xin[t % 2]
            s.dma("sp", xt, C.x_d[seq, t * 128:(t + 1) * 128, :])
            for half in range(2):
                p = C.PS[(t * 2 + half) % 2]
                for c4 in range(4):
                    c = half * 4 + c4
                    s.transpose(p[:, c4 * 128:(c4 + 1) * 128], xt[:, c * 128:(c + 1) * 128], C.ident_f)
                src = Tile(p.ap.rearrange("p (c n) -> p c n", c=4), p.buf)
                s.copy(C.hT[:, half * 4:half * 4 + 4, t * 128:(t + 1) * 128], src, e="dve")
                s.copy(C.hbf[:, half * 4:half * 4 + 4, t * 128:(t + 1) * 128], src, e="act")
        s.barrier()


def stage_store(C, seq):
    s = C.s
    with ExitStack() as st2:
        yout = [s.sbuf(f"yout{i}", [128, D], F32, stack=st2) for i in range(2)]
        for t in range(NT):
            yt = yout[t % 2]
            for half in range(2):
                p = C.PS[(t * 2 + half) % 2]
                for c4 in range(4):
                    c = half * 4 + c4
                    s.transpose(p[:, c4 * 128:(c4 + 1) * 128], C.hT[:, c, t * 128:(t + 1) * 128], C.ident_f)
                s.copy(yt[:, half * 512:(half + 1) * 512], p, e="act" if half else "dve")
            s.dma("sp", C.y_d[seq, t * 128:(t + 1) * 128, :], yt)
        s.barrier()


def rsqrt_from_psum(C, out, ps, scale, eps, tmp):
    s = C.s
    s.act(tmp, ps, AF.Sqrt, bias=eps, scale=scale)
    s.op("dve", lambda E: E.reciprocal(out.ap, tmp.ap), reads=[tmp], writes=[out])


def stage_gdn(C, seq):
    s = C.s
    cfs = C.cfs
    hT, hbf, R3, PS, PSB = C.hT, C.hbf, C.R3, C.PS, C.PSB
    with ExitStack() as st2:
        def sb(name, shape, dt):
            return s.sbuf(name, shape, dt, stack=st2)

        wab = sb("wab", [128, 8, 16], F32)
        s.dma("sp", wab, C.w_ab.rearrange("(c p) n -> p c n", p=128))
        ab = sb("ab_tok", [128, NT, 16], F32)
        p = PS[0]
        for t in range(NT):
            for c in range(8):
                s.mm(p[:, t * 16:(t + 1) * 16], hT[:, c, t * 128:(t + 1) * 128], wab[:, c, :],
                     start=(c == 0), stop=(c == 7))
        s.copy(ab, Tile(p.ap[:, 0:256].rearrange("p (t n) -> p t n", t=NT), p.buf))

        def bc8(name):
            t_ = cfs(name)
            return Tile(t_.ap.unsqueeze(1).to_broadcast([128, NT, 8]), t_.buf)

        g = sb("g_tok", [128, NT, 8], F32)
        t1 = sb("t1", [128, NT, 8], F32)
        t2 = sb("t2", [128, NT, 8], F32)
        negA = sb("negA", [128, 8], F32)
        s.act(negA, cfs("alog"), AF.Exp)
        s.ts(negA, negA, -1.0, ALU.mult)
        s.tt(t1, ab[:, :, 0:8], bc8("dtb"), ALU.add)
        s.ts(t2, t1, 60.0, ALU.min)
        s.act(t2, t2, AF.Exp)
        s.act(t2, t2, AF.Ln, bias=1.0)
        s.tt(t2, t2, t1, ALU.max)
        negA_b = Tile(negA.ap.unsqueeze(1).to_broadcast([128, NT, 8]), negA.buf)
        s.tt(g, t2, negA_b, ALU.mult)
        beta = sb("beta", [128, NT, 8], F32)
        s.act(beta, ab[:, :, 8:16], AF.Sigmoid)
        negbeta = sb("negbeta", [128, NT, 8], F32)
        s.ts(negbeta, beta, -1.0, ALU.mult)
        gc = sb("gc", [128, NT, 8], F32)
        glast = sb("glast", [128, NT, 8], F32)
        p = PS[1]
        for t in range(NT):
            s.mm(p[:, t * 8:(t + 1) * 8], C.U_f, g[:, t, :])
        s.copy(gc, Tile(p.ap[:, 0:128].rearrange("p (t n) -> p t n", t=NT), p.buf))
        p = PS[2]
        for t in range(NT):
            s.mm(p[:, t * 8:(t + 1) * 8], C.ones_f, g[:, t, :])
        s.copy(glast, Tile(p.ap[:, 0:128].rearrange("p (t n) -> p t n", t=NT), p.buf))
        negegc = sb("negegc", [128, NT, 8], F32)
        s.act(negegc, gc, AF.Exp)
        s.ts(negegc, negegc, -1.0, ALU.mult)
        d_b = sb("d_b", [128, NT, 8], F32)
        s.act(d_b, glast, AF.Exp)
        bdecl = sb("bdecl", [128, NT, 8], F32)
        s.tt(bdecl, glast, gc, ALU.subtract)
        s.act(bdecl, bdecl, AF.Exp)
        s.tt(bdecl, bdecl, beta, ALU.mult)

        wq = sb("wq", [128, 8, 384], BF16)
        wz = [sb(f"wz{i}", [128, 8, 128], BF16) for i in range(2)]
        seg = [sb(f"cinseg{i}", [128, 515], F32) for i in range(4)]
        s.memset(seg[0][:, 0:3], 0.0)
        acc = sb("acc", [128, T], F32)
        accseg = [acc.split((slice(None), slice(i * 512, (i + 1) * 512)), f"accseg{i}") for i in range(4)]
        sqb = sb("sqb", [128, T], BF16)
        VT = sqb
        sq2 = [sb(f"sq2_{i}", [128, 512], BF16) for i in range(2)]
        rsT = sb("rsT", [128, 48], F32)
        QT = sb("QT", [128, T], BF16)
        KT = sb("KT", [128, T], BF16)
        ztmp = sb("ztmp", [128, 512], BF16)
        Ktok = sb("Ktok", [128, NT, 128], BF16)
        Vtok = sb("Vtok", [128, NT, 128], BF16)
        Nb = [sb(f"Nb{i}", [128, 4, 128], BF16) for i in range(2)]
        qkT = [sb(f"qkT{i}", [128, 4, 128], BF16) for i in range(2)]
        qdT = [sb(f"qdT{i}", [128, 4, 128], BF16) for i in range(2)]
        A1 = sb("A1", [128, 4, 128], F32)
        A2 = sb("A2", [128, 4, 128], F32)
        A3 = sb("A3", [128, 4, 128], F32)
        Pm = [sb(f"Pm{i}", [128, 4, 128], BF16) for i in range(2)]
        PTm = [sb(f"PTm{i}", [128, 4, 128], BF16) for i in range(2)]
        Nbt = [sb(f"Nbt{i}", [128, 4, 128], BF16) for i in range(2)]
        S = sb("S", [128, 128], F32)
        Sb = sb("Sb", [128, 128], BF16)
        Y = sb("Y", [128, 128], BF16)
        vnew = sb("vnew", [128, 128], BF16)
        vdec = sb("vdec", [128, 128], BF16)

        def flat(t_):
            return Tile(t_.ap.rearrange("p a b -> p (a b)"), t_.buf)

        def v4(t_):
            return Tile(t_.ap.rearrange("p (a b) -> p a b", a=4), t_.buf)

        def bc_mid(t_):
            return Tile(t_.ap.unsqueeze(1).to_broadcast([128, 4, 128]), t_.buf)

        def bc_last(t_):
            return Tile(t_.ap.to_broadcast([128, 4, 128]), t_.buf)

        rsq, rsq2 = flat(A1), flat(A2)

        print("gdn sbuf bytes remaining", C.nc.sbuf_bytes_remaining)

        w_r = C.w_in_r.rearrange("(c p) h f -> p c h f", p=128)

        def load_wq(h):
            s.dma("pool", wq, w_r[:, :, h, 0:384])

        def load_wz(h):
            s.dma("pool", wz[h % 2], w_r[:, :, h, 384:512])

        def phase1(h, grp):
            n0 = grp * 4
            b = grp % 2
            tsl = slice(n0 * 128, (n0 + 4) * 128)
            gsl = g[:, n0:n0 + 4, h:h + 1]
            s.tt(A1, bc_mid(C.U_f), bc_last(gsl), ALU.mult)
            s.mm(PS[2], C.ones_f, flat(A1))
            for k in range(4):
                sl = slice((n0 + k) * 128, (n0 + k + 1) * 128)
                s.mm(PS[3][:, k * 128:(k + 1) * 128], KT[:, sl], KT[:, sl])
            for k in range(4):
                sl = slice((n0 + k) * 128, (n0 + k + 1) * 128)
                s.mm(PS[4][:, k * 128:(k + 1) * 128], KT[:, sl], QT[:, sl])
            yield
            s.tt(A1, v4(PS[2]), bc_last(gc[:, n0:n0 + 4, h:h + 1]), ALU.subtract)
            s.tt(A1, A1, bc_mid(C.NEGU_f), ALU.add)
            s.act(A2, v4(PS[2]), AF.Exp)
            s.act(A1, A1, AF.Exp)
            s.tt(qdT[b], v4(QT[:, tsl]), A2, ALU.mult)
            s.tt(A2, A1, bc_mid(C.SU_f), ALU.mult)
            s.tt(qkT[b], v4(PS[4]), A1, ALU.mult)
            s.tt(A3, v4(PS[3]), bc_last(negbeta[:, n0:n0 + 4, h:h + 1]), ALU.mult)
            s.tt(Pm[0], A3, A2, ALU.mult)
            for k in range(4):
                s.transpose(PSB[:, k * 128:(k + 1) * 128], Pm[0][:, k, :], C.ident_b)
            yield
            s.copy(PTm[0], v4(PSB[:, 0:512]), e="act")
            s.tt(A3, Pm[0], bc_mid(C.ident_f), ALU.add)
            s.copy(Nbt[0], A3, e="act")
            cur = 0
            for j in range(1, 7):
                nxt = 1 - cur
                if j < 6:
                    for k in range(4):
                        s.mm(PS[3][:, k * 128:(k + 1) * 128], PTm[cur][:, k, :], Pm[cur][:, k, :])
                for k in range(4):
                    s.mm(PS[4][:, k * 128:(k + 1) * 128], Pm[cur][:, k, :], PTm[cur][:, k, :])
                yield
                if j < 6:
                    s.copy(Pm[nxt], v4(PS[3]), e="act")
                s.copy(PTm[nxt], v4(PS[4]), e="dve")
                for k in range(4):
                    s.mm(PS[5][:, k * 128:(k + 1) * 128], PTm[nxt][:, k, :], Nbt[cur][:, k, :])
                yield
                s.tt(A3, A3, v4(PS[5]), ALU.add)
                if j < 6:
                    s.copy(Nbt[nxt], A3, e="act")
                else:
                    s.copy(Nb[b], A3, e="act")
                cur = nxt

        def chain(h, grp):
            b = grp % 2
            for k in range(4):
                n = grp * 4 + k
                sl = slice(n * 128, (n + 1) * 128)
                s.mm(PS[6][:, 0:128], KT[:, sl], Sb)
                yield
                s.stt(Y, PS[6][:, 0:128], negegc[:, n, h:h + 1], Vtok[:, n, :], ALU.mult, ALU.add)
                s.mm(PS[6][:, 128:256], Nb[b][:, k, :], Y)
                yield
                s.ts(vnew, PS[6][:, 128:256], beta[:, n, h:h + 1], ALU.mult)
                s.ts(vdec, PS[6][:, 128:256], bdecl[:, n, h:h + 1], ALU.mult)
                s.mm(PS[0][:, 0:128], Sb, qdT[b][:, k, :], start=True, stop=False)
                s.mm(PS[0][:, 0:128], vnew, qkT[b][:, k, :], start=False, stop=True)
                s.mm(PS[1][:, 0:128], Ktok[:, n, :], vdec)
                yield
                s.copy(accseg[n // 4][:, (n % 4) * 128:(n % 4 + 1) * 128], PS[0][:, 0:128], e="act")
                s.stt(S, S, d_b[:, n, h:h + 1], PS[1][:, 0:128], ALU.mult, ALU.add)
                s.copy(Sb, S, e="act")

        def run_gens(*gens):
            gens = list(gens)
            while gens:
                for gi in list(gens):
                    try:
                        next(gi)
                    except StopIteration:
                        gens.remove(gi)

        load_wq(0)
        load_wz(0)
        for h in range(8):
            w = wq
            steps = [(part, tg) for part in range(3) for tg in range(NTG)]

            def stageA(i):
                part, tg = steps[i]
                pp = PS[i % 2]
                for c in range(8):
                    s.mm(pp, w[:, c, part * 128:(part + 1) * 128], hbf[:, c, tg * 512:(tg + 1) * 512],
                         start=(c == 0), stop=(c == 7))
                s.copy(seg[tg][:, 3:515], pp, e="act")
                if tg + 1 < NTG:
                    s.copy(seg[tg + 1][:, 0:3], seg[tg][:, 512:515], e="pool")

            def stageB(i):
                part, tg = steps[i]
                a = accseg[tg]
                cwi = lambda j: C.cfs("convw", j * 24 + part * 8 + h, j * 24 + part * 8 + h + 1)
                s.ts(a, seg[tg][:, 0:512], cwi(0), ALU.mult)
                for j in range(1, 4):
                    s.stt(a, seg[tg][:, j:j + 512], cwi(j), a, ALU.mult, ALU.add)
                s.act(a, a, AF.Silu)
                if part < 2:
                    s.act(sq2[i % 2], a, AF.Square)
                else:
                    s.copy(VT[:, tg * 512:(tg + 1) * 512], a, e="act")

            def bc_rs(col0):
                t_ = rsT[:, col0:col0 + 4]
                return Tile(t_.ap.unsqueeze(2).to_broadcast([128, 4, 128]), t_.buf)

            def bcast_rs(col0):
                s.tt(A1, bc_mid(C.ident_f), bc_rs(col0), ALU.mult)
                s.mm(PS[2], C.ones_f, flat(A1))

            def stageC(i):
                part, tg = steps[i]
                if part == 2:
                    return
                for k in range(4):
                    col = part * 16 + tg * 4 + k
                    s.mm(PS[3][:, col:col + 1], sq2[i % 2][:, k * 128:(k + 1) * 128], C.ones_b[:, 0:1])
                if tg == NTG - 1:
                    cs = slice(part * 16, part * 16 + 16)
                    s.act(rsT[:, cs], PS[3][:, cs], AF.Sqrt, bias=1e-6)
                    s.op("dve", lambda E, cs=cs: E.reciprocal(rsT.ap[:, cs], rsT.ap[:, cs]), reads=[rsT], writes=[rsT])
                    for t2 in range(NTG):
                        tsl = slice(t2 * 512, (t2 + 1) * 512)
                        bcast_rs(part * 16 + t2 * 4)
                        if part == 0:
                            s.stt(QT[:, tsl], accseg[t2], 128 ** -0.5, PS[2], ALU.mult, ALU.mult)
                        else:
                            s.tt(KT[:, tsl], accseg[t2], PS[2], ALU.mult)

            def tok_major(src, dst):
                for t4 in range(4):
                    for k in range(4):
                        t = t4 * 4 + k
                        s.transpose(PSB[:, k * 128:(k + 1) * 128], src[:, t * 128:(t + 1) * 128], C.ident_b)
                    s.copy(dst[:, t4 * 4:(t4 + 1) * 4, :], v4(PSB[:, 0:512]), e="act")

            for i in range(len(steps) + 2):
                if i < len(steps):
                    stageA(i)
                if 0 <= i - 2 < len(steps):
                    stageC(i - 2)
                if 0 <= i - 1 < len(steps):
                    stageB(i - 1)
                if i == 11:
                    tok_major(KT, Ktok)
            tok_major(VT, Vtok)
            if h + 1 < 8:
                load_wq(h + 1)
                load_wz(h + 1)
            s.memset(S, 0.0)
            s.memset(Sb, 0.0)
            run_gens(phase1(h, 0))
            for grp in range(4):
                if grp + 1 < 4:
                    run_gens(phase1(h, grp + 1), chain(h, grp))
                else:
                    run_gens(chain(h, grp))
            for tg in range(NTG):
                s.act(sq2[tg % 2], accseg[tg], AF.Square)
                for k in range(4):
                    col = 32 + tg * 4 + k
                    s.mm(PS[3][:, col:col + 1], sq2[tg % 2][:, k * 128:(k + 1) * 128], C.ones_b[:, 0:1])
            s.act(rsT[:, 32:48], PS[3][:, 32:48], AF.Sqrt, bias=1e-6, scale=1.0 / 128)
            s.op("dve", lambda E: E.reciprocal(rsT.ap[:, 32:48], rsT.ap[:, 32:48]), reads=[rsT], writes=[rsT])
            for tg in range(NTG):
                tsl = slice(tg * 512, (tg + 1) * 512)
                pp = PS[tg % 2]
                for c in range(8):
                    s.mm(pp, wz[h % 2][:, c, :], hbf[:, c, tsl], start=(c == 0), stop=(c == 7))
                s.act(ztmp, pp, AF.Silu)
                bcast_rs(32 + tg * 4)
                s.tt(rsq2, accseg[tg], PS[2], ALU.mult)
                s.stt(R3[:, h, tsl], rsq2, C.cfs("normw"), ztmp, ALU.mult, ALU.mult)
        s.barrier()


def layer_norm_tg(C, st2, tg, k, scratch):
    s = C.s
    hT, hbf, PS = C.hT, C.hbf, C.PS
    sqs, mean, msq, rstd, tmp = scratch
    tsl = slice(tg * 512, (tg + 1) * 512)
    for c in range(8):
        s.copy(hbf[:, c, tsl], hT[:, c, tsl], e="act")
        s.act(sqs[:, c, :], hT[:, c, tsl], AF.Square)
    for c in range(8):
        s.mm(PS[4], C.ones_b, hbf[:, c, tsl], start=(c == 0), stop=(c == 7))
    for c in range(8):
        s.mm(PS[5], C.ones_b, sqs[:, c, :], start=(c == 0), stop=(c == 7))
    s.ts(mean, PS[4], 1.0 / D, ALU.mult)
    s.tt(msq, mean, mean, ALU.mult)
    s.stt(msq, PS[5], 1.0 / D, msq, ALU.mult, ALU.subtract)
    s.act(tmp, msq, AF.Sqrt, bias=1e-5)
    s.op("dve", lambda E: E.reciprocal(rstd.ap, tmp.ap), reads=[tmp], writes=[rstd])
    for c in range(8):
        gcol = C.cfs("lng", k * 8 + c, k * 8 + c + 1)
        bcol = C.cfs("lnb", k * 8 + c, k * 8 + c + 1)
        s.tt(tmp, hT[:, c, tsl], mean, ALU.subtract)
        s.tt(tmp, tmp, rstd, ALU.mult, e="pool")
        s.ts(hT[:, c, tsl], tmp, gcol, ALU.mult, bcol, ALU.add)
        s.copy(hbf[:, c, tsl], hT[:, c, tsl], e="act")


def ln_scratch(C, st2):
    s = C.s
    return (s.sbuf("ln_sqs", [128, 8, 512], BF16, stack=st2),
            s.sbuf("ln_mean", [128, 512], F32, stack=st2),
            s.sbuf("ln_msq", [128, 512], F32, stack=st2),
            s.sbuf("ln_rstd", [128, 512], F32, stack=st2),
            s.sbuf("ln_tmp", [128, 512], F32, stack=st2))


def stage_proj_ln(C, seq, w_d, k, src):
    s = C.s
    hT, PS = C.hT, C.PS
    with ExitStack() as st2:
        wo = s.sbuf("wo", [128, 8, 1024], BF16, stack=st2)
        s.dma("pool", wo, w_d.rearrange("(c p) n -> p c n", p=128))
        scratch = ln_scratch(C, st2)
        def proj(tg):
            tsl = slice(tg * 512, (tg + 1) * 512)
            for c in range(8):
                pp = PS[c % 4]
                for kc in range(8):
                    s.mm(pp, wo[:, kc, c * 128:(c + 1) * 128], src[:, kc, tsl], start=(kc == 0), stop=(kc == 7))
                s.stt(hT[:, c, tsl], hT[:, c, tsl], ALPHA, pp, ALU.mult, ALU.add)
        proj(0)
        for tg in range(NTG):
            if tg + 1 < NTG:
                proj(tg + 1)
            layer_norm_tg(C, st2, tg, k, scratch)
        s.barrier()


def stage_moe(C, seq, layer):
    s = C.s
    hT, hbf, R3, PS = C.hT, C.hbf, C.R3, C.PS
    hdn = R3
    with ExitStack() as st2:
        def sb(name, shape, dt):
            return s.sbuf(name, shape, dt, stack=st2)

        wr = sb("wr", [128, 8, 20], F32)
        s.dma("sp", wr, C.moe_w_r[layer].rearrange("(c p) n -> p c n", p=128))
        lg = sb("lg", [128, NT, 20], F32)
        p = PS[0]
        for t in range(NT):
            for c in range(8):
                s.mm(p[:, t * 20:(t + 1) * 20], hT[:, c, t * 128:(t + 1) * 128], wr[:, c, :],
                     start=(c == 0), stop=(c == 7))
        rb = C.cfs("rbias", layer * 20, layer * 20 + 20)
        s.tt(lg, Tile(p.ap[:, 0:NT * 20].rearrange("p (t n) -> p t n", t=NT), p.buf),
             Tile(rb.ap.unsqueeze(1).to_broadcast([128, NT, 20]), rb.buf), ALU.add)

        def red(out, in_, op):
            s.op("dve", lambda E: E.tensor_reduce(out.ap, in_.ap, AX.X, op), reads=[in_], writes=[out])

        def bl(t_, n):
            return Tile(t_.ap.unsqueeze(2).to_broadcast([128, t_.ap.shape[1], n]), t_.buf)

        lgv = lg[:, :, 0:4]
        le = sb("le", [128, NT * 4, 4], F32)
        s.copy(Tile(le.ap.rearrange("p (t g) i -> p t (g i)", t=NT), le.buf), lg[:, :, 4:20])
        mg = sb("mg", [128, NT], F32)
        eg = sb("eg", [128, NT, 4], F32)
        sg = sb("sg", [128, NT], F32)
        oh = sb("oh", [128, NT, 4], F32)
        red(mg, lgv, ALU.max)
        s.tt(eg, lgv, bl(mg, 4), ALU.subtract)
        s.tt(oh, lgv, bl(mg, 4), ALU.is_ge)
        s.act(eg, eg, AF.Exp)
        red(sg, eg, ALU.add)
        s.op("dve", lambda E: E.reciprocal(sg.ap, sg.ap), reads=[sg], writes=[sg])
        s.tt(oh, oh, bl(sg, 4), ALU.mult)
        m1 = sb("m1", [128, NT * 4], F32)
        Ee = sb("Ee", [128, NT * 4, 4], F32)
        t1 = sb("mt1", [128, NT * 4, 4], F32)
        red(m1, le, ALU.max)
        s.tt(Ee, le, bl(m1, 4), ALU.subtract)
        s.tt(t1, le, bl(m1, 4), ALU.is_ge)
        s.act(Ee, Ee, AF.Exp)
        s.stt(t1, t1, -1e30, le, ALU.mult, ALU.add)
        m2 = sb("m2", [128, NT * 4], F32)
        red(m2, t1, ALU.max)
        s.tt(t1, le, bl(m2, 4), ALU.is_ge)
        s.tt(Ee, Ee, t1, ALU.mult)
        red(m2, Ee, ALU.add)
        s.op("dve", lambda E: E.reciprocal(m2.ap, m2.ap), reads=[m2], writes=[m2])
        ohf = Tile(oh.ap.rearrange("p t g -> p (t g)"), oh.buf)
        s.tt(m2, m2, ohf, ALU.mult)
        gate = sb("gate", [128, NT * 4, 4], F32)
        s.tt(gate, Ee, bl(m2, 4), ALU.mult)
        gate_v = Tile(gate.ap.rearrange("p (t g) i -> p t (g i)", t=NT), gate.buf)
        gateT = sb("gateT", [16, T], BF16)
        for tg in range(NTG):
            for k in range(4):
                t = tg * 4 + k
                s.transpose(PS[1][0:16, k * 128:(k + 1) * 128], gate_v[:, t, :], C.ident_f)
            s.copy(gateT[:, tg * 512:(tg + 1) * 512], PS[1][0:16, :], e="act")
        selE = sb("selE", [16, 16, 128], BF16)
        idv = C.ident_f[0:16, 0:16]
        s.copy(selE, Tile(idv.ap.unsqueeze(2).to_broadcast([16, 16, 128]), idv.buf))

        for c in range(8):
            s.ts(hT[:, c, :], hT[:, c, :], ALPHA, ALU.mult, e=("pool" if c % 2 else "dve"))
        wgu = [sb(f"wgu{i}", [128, 2, 8, 256], BF16) for i in range(2)]
        wd = sb("wd", [128, 4, 2, 1024], BF16)
        sgt = sb("sgt", [128, 512], F32)
        tmu = sb("tmu", [128, 512], F32)
        gbanks = [PS[4], C.PSB.bitcast(F32)]
        for e in range(16):
            wb = wgu[e % 2]
            s.dma("pool", wb[:, 0], C.moe_w_gate[layer, e].rearrange("(c p) f -> p c f", p=128))
            s.dma("pool", wb[:, 1], C.moe_w_up[layer, e].rearrange("(c p) f -> p c f", p=128))
            s.dma("pool", wd[:, e % 4], C.moe_w_down[layer, e].rearrange("(c p) n -> p c n", p=128))
            for tg in range(NTG):
                tsl = slice(tg * 512, (tg + 1) * 512)
                gbk = gbanks[tg % 2]
                s.mm(gbk, selE[:, e, :], gateT[:, tsl])
                for fc in range(2):
                    for c in range(8):
                        s.mm(PS[2 * fc], wb[:, 0, c, fc * 128:(fc + 1) * 128], hbf[:, c, tsl],
                             start=(c == 0), stop=(c == 7))
                    for c in range(8):
                        s.mm(PS[2 * fc + 1], wb[:, 1, c, fc * 128:(fc + 1) * 128], hbf[:, c, tsl],
                             start=(c == 0), stop=(c == 7))
                    s.act(sgt, PS[2 * fc], AF.Silu)
                    s.tt(tmu, sgt, PS[2 * fc + 1], ALU.mult)
                    s.tt(hdn[:, (e % 4) * 2 + fc, tsl], tmu, gbk, ALU.mult)
            if e % 4 == 3:
                for tg in range(NTG):
                    tsl = slice(tg * 512, (tg + 1) * 512)
                    for c in range(8):
                        pp = PS[5 + c % 2]
                        for k in range(8):
                            s.mm(pp, wd[:, k // 2, k % 2, c * 128:(c + 1) * 128], hdn[:, k, tsl],
                                 start=(k == 0), stop=(k == 7))
                        s.tt(hT[:, c, tsl], hT[:, c, tsl], pp, ALU.add)
        s.barrier()
    with ExitStack() as st2:
        scratch = ln_scratch(C, st2)
        for tg in range(NTG):
            layer_norm_tg(C, st2, tg, 2 * layer + 1, scratch)
        s.barrier()


def host_pack_nsa(inp):
    cn = Pack()
    i = np.arange(128)
    Dn = np.concatenate([(128 * j + i[None, :] - i[:, None]) for j in range(2)], axis=1)
    cn.add("Dnear", Dn)
    Dc = np.full((128, 128), -1.0)
    for m in range(24):
        Dc[m] = i - 16 * (m - 16) - 31
    Dc[24] = 1000.0
    cn.add("Dcmp", Dc)
    Wc = np.zeros((128, 16, 128))
    for c in range(16):
        for n in range(128):
            n1 = n - 8 * c
            m = n1 + 16 if -16 <= n1 <= 7 else (24 if n1 < -16 else 25)
            if n == 127:
                m = 25
            Wc[m, c, n] = 1.0
    cn.add("Wc", Wc)
    WM4 = np.where(i[:, None] > i[None, :], 0.0, NEG)
    cn.add("WM4x4", np.tile(WM4, (1, 4)))
    Ee = np.zeros((128, 16, 128))
    for t in range(16):
        for k in range(128):
            Ee[2 * t + (k >= 64), t, k] = 1.0
    cn.add("Eexp", Ee)
    OV = np.zeros((128, 33))
    for n in range(127):
        for j in range(32):
            if 16 * n < 64 * j + 64 and 16 * n + 31 >= 64 * j:
                OV[n, j] = 1.0
        OV[n, 32] = 1.0
    cn.add("OVL", OV)
    valid = np.zeros((128, 16, 32)); addm = np.zeros((128, 16, 32))
    for c in range(16):
        for ii in range(128):
            jq = 2 * c + (ii >= 64)
            for j in range(32):
                forced = (j == 0) or (j == jq) or (j == jq - 1)
                if forced:
                    addm[ii, c, j] = 1e9
                elif j > jq:
                    addm[ii, c, j] = -1e9
                else:
                    valid[ii, c, j] = 1.0
    cn.add("valid", valid)
    cn.add("addm", addm)
    SP = np.zeros((128, 24, 128))
    for g in range(4):
        for p in range(2):
            for br in range(3):
                for slot in range(2):
                    r = (g * 4 + p * 2 + slot) * 3 + br
                    SP[r, (g * 2 + p) * 3 + br, slot * 64:(slot + 1) * 64] = 1.0
    cn.add("SelPair", SP)
    cn.add("relb", rep(inp["rel_bias"].reshape(-1)))
    return cn


def host_weights_nsa(inp):
    w = {}
    wkv = inp["nsa_w_kv"].reshape(1024, 6, 4, 64)
    w["nsa_w_raw"] = np.ascontiguousarray(wkv[:, [0, 1]].reshape(1024, 512))
    kd = wkv[:, [2, 4]]
    w["nsa_w_kdup"] = np.ascontiguousarray(np.repeat(kd[:, :, :, None, :], 2, axis=3).reshape(1024, 2, 512))
    w["nsa_w_vtok"] = np.ascontiguousarray(wkv[:, [3, 5]].reshape(1024, 2, 256))
    w["nsa_w_in"] = np.ascontiguousarray(inp["nsa_w_in"][0])
    w["nsa_w_o"] = np.ascontiguousarray(inp["nsa_w_o"][0])
    w1 = np.stack([inp["cmp_k_w1"], inp["cmp_v_w1"]]).reshape(2, 32, 64, 128).transpose(0, 2, 1, 3)
    w["cmp_w1dup"] = np.ascontiguousarray(np.concatenate([w1, w1], axis=1))
    w["cmp_w2k_dup"] = np.ascontiguousarray(np.concatenate([inp["cmp_k_w2"], inp["cmp_k_w2"]], axis=1))
    w["cmp_w2v"] = np.ascontiguousarray(inp["cmp_v_w2"])
    pT = np.stack([inp["cmp_k_pos"].T, inp["cmp_v_pos"].T])
    w["cmp_posT"] = np.ascontiguousarray(np.concatenate([pT, pT], axis=1))
    return w


def nsa_declare(C, nc, dram_in):
    C.cn_d = dram_in("cn", [128, C.cn_n])
    C.nsa_w_raw = dram_in("nsa_w_raw", [1024, 512])
    C.nsa_w_kdup = dram_in("nsa_w_kdup", [1024, 2, 512])
    C.nsa_w_vtok = dram_in("nsa_w_vtok", [1024, 2, 256])
    C.nsa_w_in = dram_in("nsa_w_in", [1024, 1072])
    C.nsa_w_o = dram_in("nsa_w_o", [1024, 1024])
    C.cmp_w1dup = dram_in("cmp_w1dup", [2, 128, 32, 128])
    C.cmp_w2k_dup = dram_in("cmp_w2k_dup", [128, 128])
    C.cmp_w2v = dram_in("cmp_w2v", [128, 64])
    C.cmp_posT = dram_in("cmp_posT", [2, 128, 32])
    C.bt_scr = nc.dram_tensor("bt_scr", [128, 4, 2, 512], BF16, kind="Internal").ap()
    C.pat_scr = nc.dram_tensor("pat_scr", [32, 4, 512], BF16, kind="Internal").ap()


def cn_slice(C, name):
    o0, o1 = C.cn_off[name]
    return C.cn_d[:, o0:o1]


SLOT_HEADS = (0, 2, 1, 3)


def stage_nsa_setup(C):
    s = C.s
    PS = C.PS
    los = t5_lo_thresholds()
    with ExitStack() as st2:
        def sb(name, shape, dt):
            return s.sbuf(name, shape, dt, stack=st2)
        relb = sb("relb", [128, 512], F32)
        s.dma("sp", relb, cn_slice(C, "relb"))
        negD = sb("negD", [128, 31 * 16], F32)
        s.tt(negD, relb[:, 0:31 * 16], relb[:, 16:512], ALU.subtract)
        for which in ("near", "cmp"):
            W = 256 if which == "near" else 128
            NP = 128 if which == "near" else 26
            Dt_ = sb("D" + which, [128, W], F32)
            s.dma("sp", Dt_, cn_slice(C, "Dnear" if which == "near" else "Dcmp"))
            accs_all = sb("accs" + which, [128, 16, W], F32)
            acch = [accs_all.split((slice(0, NP), h, slice(None)), f"acc{which}{h}") for h in range(16)]
            Lts = [sb(f"Lt{which}{i}", [128, W], F32) for i in range(2)]
            for h in range(16):
                s.memset(acch[h], 0.0, e=("pool" if h % 2 else "dve"))
            for b in range(1, 33):
                Lt = Lts[b % 2]
                if b < 32:
                    s.ts(Lt[0:NP], Dt_[0:NP], float(los[b - 1]), ALU.is_lt)
                else:
                    s.ts(Lt[0:NP], Dt_[0:NP], 0.0, ALU.is_lt)
                for h in range(16):
                    if b < 32:
                        s.stt(acch[h], Lt[0:NP], negD[0:NP, (b - 1) * 16 + h:(b - 1) * 16 + h + 1], acch[h], ALU.mult, ALU.add)
                    else:
                        s.stt(acch[h], Lt[0:NP], NEG, acch[h], ALU.mult, ALU.add)
            if which == "near":
                bt = sb("bt_b", [128, 4, 2, 512], BF16)
                for g in range(4):
                    for sl_, hh in enumerate(SLOT_HEADS):
                        for j in range(2):
                            s.copy(bt[:, g, j, sl_ * 128:(sl_ + 1) * 128], acch[4 * g + hh][:, j * 128:(j + 1) * 128],
                                   e=("act" if j else "dve"))
                s.dma("sp", C.bt_scr, bt)
            else:
                pt = sb("pat_b", [32, 4, 512], BF16)
                s.memset(pt, 0.0)
                for g in range(4):
                    for sl_, hh in enumerate(SLOT_HEADS):
                        s.copy(pt[0:26, g, sl_ * 128:(sl_ + 1) * 128], acch[4 * g + hh])
                s.dma("sp", C.pat_scr, pt)
        w1 = sb("w1s", [128, 32, 128], BF16)
        posT = sb("posTs", [128, 2, 32], BF16)
        s.dma("pool", posT, C.cmp_posT.rearrange("k p l -> p k l"))
        for kv in range(2):
            s.dma("pool", w1, C.cmp_w1dup[kv])
            for l in range(32):
                s.mm(PS[0][:, kv:kv + 1], w1[0:64, l, :], posT[0:64, kv, l:l + 1], start=(l == 0), stop=(l == 31))
        s.copy(C.pbias, PS[0][:, 0:2])
        s.barrier()


def stage_kv(C, seq):
    s = C.s
    hbf, R3, PS = C.hbf, C.R3, C.PS
    C.kv_stack = ExitStack()
    st = C.kv_stack

    def sbp(name, shape, dt):
        return s.sbuf(name, shape, dt, stack=st)
    C.ksT2 = sbp("ksT2", [128, 4, T], BF16)
    C.kwT2 = sbp("kwT2", [128, 4, T], BF16)
    C.vs_tok = sbp("vs_tok", [128, NT, 256], BF16)
    C.vw_tok = sbp("vw_tok", [128, NT, 256], BF16)
    C.sgT = sbp("sgT", [48, T], BF16)
    C.kcT2 = sbp("kcT2", [128, 4, 128], BF16)
    C.vc = sbp("vc", [128, 4, 64], BF16)
    raw = R3
    with ExitStack() as st2:
        def sb(name, shape, dt):
            return s.sbuf(name, shape, dt, stack=st2)
        wk = sb("wkv_a", [128, 8, 512], BF16)
        s.dma("pool", wk, C.nsa_w_raw.rearrange("(c p) n -> p c n", p=128))
        for ch in range(4):
            for tg in range(NTG):
                tsl = slice(tg * 512, (tg + 1) * 512)
                pp = PS[tg % 2]
                for c in range(8):
                    s.mm(pp, wk[:, c, ch * 128:(ch + 1) * 128], hbf[:, c, tsl], start=(c == 0), stop=(c == 7))
                s.copy(raw[:, ch, tsl], pp, e=("act" if tg % 2 else "dve"))
        w1 = sb("w1c", [128, 32, 128], BF16)
        w2k = sb("w2k", [128, 128], BF16)
        w2v = sb("w2v", [128, 64], BF16)
        hid = sb("hidc", [128, 128], BF16)
        s.dma("pool", w2k, C.cmp_w2k_dup)
        s.dma("pool", w2v, C.cmp_w2v)
        s.memset(C.kcT2, 0.0)
        s.memset(C.vc, 0.0)
        for kv in range(2):
            s.dma("pool", w1, C.cmp_w1dup[kv])
            for g in range(4):
                po = (g % 2) * 64
                ch = kv * 2 + g // 2
                for l in range(32):
                    rhs = Tile(raw.ap[po:po + 64, ch, l:l + 16 * 126 + 1:16], raw.buf)
                    s.mm(PS[2][:, 0:127], w1[po:po + 64, l, :], rhs, start=(l == 0), stop=(l == 31))
                s.act(hid[:, 0:127], PS[2][:, 0:127], AF.Silu, bias=C.pbias[:, kv:kv + 1])
                if kv == 0:
                    s.mm(PS[3][:, 0:127], w2k, hid[:, 0:127])
                    s.copy(C.kcT2[:, g, 0:127], PS[3][:, 0:127])
                else:
                    s.mm(PS[3][0:127, 0:64], hid[:, 0:127], w2v)
                    s.copy(C.vc[0:127, g, :], PS[3][0:127, 0:64])
        for part, dst in ((0, C.ksT2), (1, C.kwT2)):
            s.dma("pool", wk, C.nsa_w_kdup.rearrange("(c p) k n -> p c k n", p=128)[:, :, part, :])
            for g in range(4):
                for tg in range(NTG):
                    tsl = slice(tg * 512, (tg + 1) * 512)
                    pp = PS[tg % 2]
                    for c in range(8):
                        s.mm(pp, wk[:, c, g * 128:(g + 1) * 128], hbf[:, c, tsl], start=(c == 0), stop=(c == 7))
                    s.copy(dst[:, g, tsl], pp, e=("act" if tg % 2 else "dve"))
        wv = Tile(wk.ap[:, :, 0:256], wk.buf)
        for part, dst in ((0, C.vs_tok), (1, C.vw_tok)):
            s.dma("pool", wv, C.nsa_w_vtok.rearrange("(c p) k n -> p c k n", p=128)[:, :, part, :])
            for t in range(NT):
                pp = PS[t % 2]
                for c in range(8):
                    s.mm(pp[:, 0:256], hbf[:, c, t * 128:(t + 1) * 128], wv[:, c, :], start=(c == 0), stop=(c == 7))
                s.copy(dst[:, t, :], pp[:, 0:256], e=("act" if t % 2 else "dve"))
        s.barrier()
    with ExitStack() as st2:
        wi = s.sbuf("wi", [128, 8, 1072], BF16, stack=st2)
        s.dma("pool", wi, C.nsa_w_in.rearrange("(c p) n -> p c n", p=128))
        for ch in range(9):
            M = 128 if ch < 8 else 48
            for tg in range(NTG):
                tsl = slice(tg * 512, (tg + 1) * 512)
                pp = PS[tg % 2]
                for c in range(8):
                    s.mm(pp[0:M, :], wi[:, c, ch * 128:ch * 128 + M], hbf[:, c, tsl], start=(c == 0), stop=(c == 7))
                if ch < 8:
                    s.act(R3[:, ch, tsl], pp, AF.Copy, scale=0.125)
                else:
                    s.act(C.sgT[:, tsl], pp[0:48, :], AF.Sigmoid)
        s.barrier()


def stage_nsa(C, seq):
    s = C.s
    PS, PSB, R3 = C.PS, C.PSB, C.R3
    S_ps = [PS[0], PS[1]]
    O_ps = {"cmp": PS[2], "sel": PS[3], "win": PS[4]}
    G2, IMP = PS[5], PS[6]
    arena = C.hbf.ap.rearrange("p a b -> p (a b)")
    off = [0]

    def carve(np_, ncols, dt=BF16):
        n2 = ncols * (2 if dt == F32 else 1)
        a = arena[0:np_, off[0]:off[0] + n2]
        off[0] += n2
        if dt == F32:
            a = a.bitcast(F32)
        return Tile(a, Buf())
    BT = carve(128, 4 * 2 * 512)
    Pat = carve(32, 4 * 512)
    Wc = carve(32, 16 * 128)
    WM4 = carve(128, 512)
    Eexp = carve(32, 16 * 128)
    SelP = carve(48, 24 * 128)
    OVL = carve(128, 33 + 1, F32)
    valid = carve(128, 512, F32)
    addm = carve(128, 512, F32)
    assert off[0] <= 16384, off[0]
    s.dma("sp", BT, C.bt_scr.rearrange("p g j n -> p (g j n)"))
    s.dma("sp", Pat, C.pat_scr.rearrange("p g n -> p (g n)"))
    s.dma("pool", Wc, cn_slice(C, "Wc")[0:32, :])
    s.dma("pool", WM4, cn_slice(C, "WM4x4"))
    s.dma("pool", Eexp, cn_slice(C, "Eexp")[0:32, :])
    s.dma("pool", SelP, cn_slice(C, "SelPair")[0:48, :])
    s.dma("sp", OVL[:, 0:33], cn_slice(C, "OVL"))
    s.dma("sp", valid, cn_slice(C, "valid"))
    s.dma("sp", addm, cn_slice(C, "addm"))
    BTv = Tile(BT.ap.rearrange("p (g j n) -> p g j n", g=4, j=2), BT.buf)
    Patv = Tile(Pat.ap.rearrange("p (g n) -> p g n", g=4), Pat.buf)
    Wcv = Tile(Wc.ap.rearrange("p (c n) -> p c n", c=16), Wc.buf)
    Eev = Tile(Eexp.ap.rearrange("p (t k) -> p t k", t=16), Eexp.buf)
    SelPv = Tile(SelP.ap.rearrange("p (a n) -> p a n", a=24), SelP.buf)
    validv = Tile(valid.ap.rearrange("p (c j) -> p c j", c=16), valid.buf)
    addmv = Tile(addm.ap.rearrange("p (c j) -> p c j", c=16), addm.buf)
    qblk = [[Tile(R3.ap[:, 2 * g:2 * g + 2, c * 128:(c + 1) * 128], Buf()) for g in range(4)] for c in range(16)]
    with ExitStack() as st2:
        def sb(name, shape, dt):
            return s.sbuf(name, shape, dt, stack=st2)
        Pb = [sb(f"Pb{i}", [128, 512], BF16) for i in range(3)]
        Pf = sb("Pf", [128, 512], F32)
        rd = sb("rd", [128, 256], F32)
        fac = sb("fac", [128, 256], F32)
        con = sb("con", [128, 256], F32)
        oacc = sb("oacc", [128, 256], F32)
        rec4 = sb("rec4", [128, 4], F32)
        impn = sb("impn", [128, 32], F32)
        imp2 = sb("imp2", [128, 32], F32)
        m8a = sb("m8a", [128, 8], F32)
        m8b = sb("m8b", [128, 8], F32)
        nsel = sb("nsel", [128, 32], BF16)
        nselT = sb("nselT", [32, 4, 128], BF16)
        pbi = [0]

        qA = [sb(f"qA{i}", [128, 2, 128], BF16) for i in range(2)]
        qB = [sb(f"qB{i}", [128, 2, 128], BF16) for i in range(2)]
        for i in range(2):
            s.memset(qA[i], 0.0)
            s.memset(qB[i], 0.0)
        qcur = [None, None]

        def qk(Sps, kT2, g, t, c, first_start):
            ksl = slice(t * 128, (t + 1) * 128)
            s.mm(Sps[:, 0:256], kT2[:, g, ksl], qcur[0], start=first_start, stop=False, skip_group_check=True)
            s.mm(Sps[:, 256:512], kT2[:, g, ksl], qcur[1], start=False, stop=True, skip_group_check=True)

        def pv(O, P, vtile, np_, first):
            for half in range(2):
                rows = slice(half * 64, half * 64 + 64)
                pc = slice(half * 256, half * 256 + 256)
                s.mm(O[rows, 0:256], vtile, P[0:np_, pc], start=first, stop=False, skip_group_check=True)
                s.mm(O[rows, 256:512], C.ones_b[0:np_, 0:64], P[0:np_, pc], start=False, stop=False, skip_group_check=True)

        def combine(br_i, key, c, g, oacc, gt):
            O = O_ps[key]
            s.ts(rd, O[:, 256:512], 1e-30, ALU.max)
            s.op("dve", lambda E: E.reciprocal(rd.ap, rd.ap), reads=[rd], writes=[rd])
            s.tt(fac, rd, gt[:, br_i * 256:(br_i + 1) * 256], ALU.mult)
            if br_i == 0:
                s.tt(oacc, O[:, 0:256], fac, ALU.mult)
            else:
                s.tt(con, O[:, 0:256], fac, ALU.mult)
                s.tt(oacc, oacc, con, ALU.add, e="pool")

        blocks = [(c, g) for c in range(16) for g in range(4)]
        nsel2 = [nsel, sb("nsel_b", [128, 32], BF16)]
        nselT2 = [nselT, sb("nselT_b", [32, 4, 128], BF16)]
        oacc2 = [oacc, sb("oacc_b", [128, 256], F32)]
        Pc = sb("Pc", [128, 512], BF16)

        gate3 = [sb(f"gate3_{i}", [128, 768], F32) for i in range(2)]

        def prep(b):
            c, g = blocks[b]
            q = qblk[c][g]
            s.copy(qA[b % 2][0:64], q[0:64], e="act")
            s.copy(qB[b % 2][64:128], q[64:128], e="act")
            sg = C.sgT[:, c * 128:(c + 1) * 128]
            for br in (1, 2):
                for p_ in range(2):
                    s.mm(G2[:, (br - 1) * 256 + p_ * 128:(br - 1) * 256 + (p_ + 1) * 128],
                         SelPv[:, (g * 2 + p_) * 3 + br, :], sg)
            for p_ in range(2):
                s.mm(IMP[:, 256 + p_ * 128:256 + (p_ + 1) * 128], SelPv[:, (g * 2 + p_) * 3 + 0, :], sg)
            s.copy(gate3[b % 2][:, 256:768], G2, e="act")
            s.copy(gate3[b % 2][:, 0:256], IMP[:, 256:512], e="act")

        def qk2(Sps, kT2, g, t, qa, qb, first_start):
            ksl = slice(t * 128, (t + 1) * 128)
            s.mm(Sps[:, 0:256], kT2[:, g, ksl], qa, start=first_start, stop=False, skip_group_check=True)
            s.mm(Sps[:, 256:512], kT2[:, g, ksl], qb, start=False, stop=True, skip_group_check=True)

        def cmpA(b):
            c, g = blocks[b]
            qa, qb = qA[b % 2], qB[b % 2]
            Sps, P = S_ps[0], Pc
            s.mm(Sps, Wcv[0:26, c, :], Patv[0:26, g, :], start=True, stop=False, skip_group_check=True)
            s.mm(Sps[:, 0:256], C.kcT2[:, g, :], qa, start=False, stop=False, skip_group_check=True)
            s.mm(Sps[:, 256:512], C.kcT2[:, g, :], qb, start=False, stop=True, skip_group_check=True)
            s.act(Pf, Sps, AF.Exp)
            s.copy(P, Pf)
            pv(O_ps["cmp"], P, C.vc[:, g, :], 128, True)
            for sl_ in range(4):
                s.mm(IMP[:, sl_ * 33:(sl_ + 1) * 33], Pf[:, sl_ * 128:(sl_ + 1) * 128], OVL[:, 0:33])
            impv = Tile(IMP.ap[:, 0:132].rearrange("p (s n) -> p s n", s=4), IMP.buf)
            s.ts(rec4, impv[:, :, 32], 1e-30, ALU.max)
            s.op("dve", lambda E: E.reciprocal(rec4.ap, rec4.ap), reads=[rec4], writes=[rec4])
            s.ts(impn, impv[:, 0, 0:32], rec4[:, 0:1], ALU.mult)
            for sl_ in range(1, 4):
                s.stt(impn, impv[:, sl_, 0:32], rec4[:, sl_:sl_ + 1], impn, ALU.mult, ALU.add)
            s.tt(impn, impn, validv[:, c, :], ALU.mult)
            s.tt(impn, impn, addmv[:, c, :], ALU.add)
            s.op("dve", lambda E: E.max(out=m8a.ap, in_=impn.ap), reads=[impn], writes=[m8a])
            s.op("dve", lambda E: E.match_replace(out=imp2.ap, in_to_replace=m8a.ap, in_values=impn.ap, imm_value=-3e9),
                 reads=[m8a, impn], writes=[imp2])
            s.op("dve", lambda E: E.max(out=m8b.ap, in_=imp2.ap), reads=[imp2], writes=[m8b])
            s.ts(imp2, impn, m8b[:, 7:8], ALU.is_ge)
            s.ts(nsel2[b % 2], imp2, -NEG, ALU.mult, NEG, ALU.add)

        def cmpB(b):
            c, g = blocks[b]
            s.transpose(PSB[0:32, 0:128], nsel2[b % 2], C.ident_b)
            s.copy(nselT2[b % 2], Tile(PSB.ap[0:32, 0:128].unsqueeze(1).to_broadcast([32, 4, 128]), PSB.buf), e="act")
            combine(0, "cmp", c, g, oacc2[b % 2], gate3[b % 2])

        def block_tiles(b):
            c, g = blocks[b]
            qa, qb = qA[b % 2], qB[b % 2]
            nT = nselT2[b % 2]
            nT_f = Tile(nT.ap.rearrange("p a b -> p (a b)"), nT.buf)
            tiles = []
            t0 = max(0, c - 4)
            for t in range(t0, c + 1):
                j = c - t

                def sc(Sps, t=t, j=j):
                    if j <= 1:
                        s.mm(Sps, C.ident_b, BTv[:, g, j, :], start=True, stop=False, skip_group_check=True)
                        qk2(Sps, C.kwT2, g, t, qa, qb, False)
                    elif j == 4:
                        s.mm(Sps, C.ident_b, WM4, start=True, stop=False, skip_group_check=True)
                        qk2(Sps, C.kwT2, g, t, qa, qb, False)
                    else:
                        qk2(Sps, C.kwT2, g, t, qa, qb, True)
                tiles.append((sc, "win", C.vw_tok[:, t, g * 64:(g + 1) * 64], t == t0))
            for t in range(c + 1):
                j = c - t

                def sc(Sps, t=t, j=j):
                    s.mm(Sps, Eev[0:32, t, :], nT_f, start=True, stop=False, skip_group_check=True)
                    qk2(Sps, C.ksT2, g, t, qa, qb, False)
                    if j <= 1:
                        s.mm(Sps, C.ident_b, BTv[:, g, j, :], start=False, stop=True, skip_group_check=True)
                tiles.append((sc, "sel", C.vs_tok[:, t, g * 64:(g + 1) * 64], t == 0))
            return tiles

        def run_block(b):
            c, g = blocks[b]
            tiles = block_tiles(b)
            n = len(tiles)
            alloc = [(S_ps[i % 2], Pb[i % 3]) for i in range(n)]
            tiles[0][0](alloc[0][0])
            for i in range(n):
                if i + 1 < n:
                    tiles[i + 1][0](alloc[i + 1][0])
                Sps, P = alloc[i]
                s.act(P, Sps, AF.Exp)
                pv(O_ps[tiles[i][1]], P, tiles[i][2], 128, tiles[i][3])
                if i == 0 and b + 1 < len(blocks):
                    prep(b + 1)
                    cmpA(b + 1)
            combine(2, "win", c, g, oacc2[b % 2], gate3[b % 2])
            combine(1, "sel", c, g, oacc2[b % 2], gate3[b % 2])
            oa = oacc2[b % 2]
            s.copy(qblk[c][g], Tile(oa.ap.rearrange("p (a b) -> p a b", a=2), oa.buf), e="pool")

        prep(0)
        cmpA(0)
        cmpB(0)
        for b in range(len(blocks)):
            run_block(b)
            if b + 1 < len(blocks):
                cmpB(b + 1)
        s.barrier()
    C.kv_stack.close()


def kernel(**inputs):
    from concourse.bass_utils import run_bass_kernel_spmd
    inp = {k: np.asarray(v) for k, v in inputs.items()}
    x = inp["x"]
    ncores = 8
    nseq = x.shape[0] // ncores
    cf = host_pack(inp)
    cn = host_pack_nsa(inp)
    build_program.cf_n = cf.n
    build_program.cf_off = cf.off
    build_program.cn_n = cn.n
    build_program.cn_off = cn.off
    w = host_weights(inp)
    w.update(host_weights_nsa(inp))
    w["cf"] = cf.build()
    w["cn"] = cn.build()
    w = {k: np.ascontiguousarray(v, dtype=np.float32) for k, v in w.items()}
    nc = bass.Bass("TRN2", target_bir_lowering=False)
    build_program(nc, nseq, "ABCDEFGHI")
    maps = []
    for i in range(ncores):
        m = dict(w)
        m["x"] = np.ascontiguousarray(x[i * nseq:(i + 1) * nseq], dtype=np.float32)
        maps.append(m)
    res = run_bass_kernel_spmd(nc, maps, core_ids=list(range(ncores)))
    return np.concatenate([np.asarray(res.results[i]["y"]) for i in range(ncores)], axis=0).astype(np.float32)
```
